# Optimizing a Trainium2 kernel written in Bass

```python
import math
import jax, jax.numpy as jnp
from jax import lax
import numpy as np

D_MODEL = 2048
BATCH = 2
SEQ = 8192
DEPTH = 1

CTX_LEN = 256
GRID_W = 64
EPS = 1e-6

MLA_HEADS = 8
MLA_Q_RANK = 512
MLA_KV_RANK = 256
MLA_NOPE = 128
MLA_ROPE = 64
MLA_V = 128
MLA_SCALE = (MLA_NOPE + MLA_ROPE) ** -0.5
ROPE_BASE = 10000.0
Q_BLOCK = 128

DN_HEADS = 8
DN_DK = 128
DN_DV = 128
DN_QKV = DN_HEADS * (2 * DN_DK + DN_DV)
CONV_W = 5
CHUNK = 64

PEER_HEADS = 8
PEER_KEYS = 128
PEER_EXPERTS = PEER_KEYS * PEER_KEYS
PEER_QDIM = 256
PEER_TOPK = 16
PEER_BLOCK = 64

IN_SIZES = (MLA_Q_RANK, MLA_KV_RANK, MLA_ROPE, DN_QKV, DN_HEADS * DN_DV,
            DN_HEADS, DN_HEADS, DN_HEADS, DN_HEADS, D_MODEL, D_MODEL)
D_IN = sum(IN_SIZES)
IN_POINTS = tuple(int(p) for p in np.cumsum(IN_SIZES)[:-1])

kernel_name = 'hybrid_mla_gdn_peer_block'


def rmsnorm(x, w):
    xf = x.astype(jnp.float32)
    y = xf * lax.rsqrt(jnp.mean(xf * xf, axis=-1, keepdims=True) + EPS)
    return (y * w.astype(jnp.float32)).astype(x.dtype)


def modulate(x, w, shift, scale):
    return rmsnorm(x, w) * (1 + scale) + shift


def l2norm(x):
    xf = x.astype(jnp.float32)
    return xf * lax.rsqrt(jnp.sum(xf * xf, axis=-1, keepdims=True) + EPS)


def grid_angles(n):
    n_rows = n // GRID_W
    rows = jnp.repeat(jnp.arange(n_rows, dtype=jnp.float32), GRID_W)
    cols = jnp.tile(jnp.arange(GRID_W, dtype=jnp.float32), n_rows)
    axis_dim = MLA_ROPE // 2
    inv_freq = ROPE_BASE ** (-jnp.arange(0, axis_dim, 2, dtype=jnp.float32) / axis_dim)
    return rows[:, None] * inv_freq, cols[:, None] * inv_freq


def rotate(x, ang):
    x1, x2 = jnp.split(x.astype(jnp.float32), 2, axis=-1)
    cos, sin = jnp.cos(ang), jnp.sin(ang)
    return jnp.concatenate([x1 * cos - x2 * sin, x1 * sin + x2 * cos], axis=-1).astype(x.dtype)


def axial_rope(x, ang_r, ang_c):
    xr, xc = jnp.split(x, 2, axis=-1)
    return jnp.concatenate([rotate(xr, ang_r), rotate(xc, ang_c)], axis=-1)


def mla_heads(c_q, c_kv, q_norm_w, kv_norm_w, w_uq, w_ukv):
    b, n = c_q.shape[:2]
    q = (rmsnorm(c_q, q_norm_w) @ w_uq).reshape(b, n, MLA_HEADS, MLA_NOPE + MLA_ROPE)
    kv = (rmsnorm(c_kv, kv_norm_w) @ w_ukv).reshape(b, n, MLA_HEADS, MLA_NOPE + MLA_V)
    return q[..., :MLA_NOPE], q[..., MLA_NOPE:], kv[..., :MLA_NOPE], kv[..., MLA_NOPE:]


def mla_attend(q_nope, q_rope, k_nope, k_rope, v):
    s = (jnp.einsum('bthd,blhd->bhtl', q_nope, k_nope)
         + jnp.einsum('bthr,blr->bhtl', q_rope, k_rope))
    p = jax.nn.softmax(s.astype(jnp.float32) * MLA_SCALE, axis=-1).astype(v.dtype)
    o = jnp.einsum('bhtl,blhd->bthd', p, v)
    return o.reshape(o.shape[0], o.shape[1], MLA_HEADS * MLA_V)


def short_conv(x, w):
    c = x.shape[-1]
    return lax.conv_general_dilated(
        x, w[:, None, :].astype(x.dtype), window_strides=(1,),
        padding=[(CONV_W // 2, CONV_W // 2)],
        dimension_numbers=('NWC', 'WIO', 'NWC'), feature_group_count=c)


def dn_qkv(qkv, conv_w):
    b, n = qkv.shape[:2]
    y = jax.nn.silu(short_conv(qkv, conv_w))
    q, k, v = jnp.split(y, [DN_HEADS * DN_DK, 2 * DN_HEADS * DN_DK], axis=-1)
    q = l2norm(q.reshape(b, n, DN_HEADS, DN_DK)) * DN_DK ** -0.5
    k = l2norm(k.reshape(b, n, DN_HEADS, DN_DK))
    v = v.reshape(b, n, DN_HEADS, DN_DV).astype(jnp.float32)
    return q, k, v


def dn_decay(a, beta_logit, a_log, dt_bias):
    g = -jnp.exp(a_log.astype(jnp.float32)) * jax.nn.softplus(a.astype(jnp.float32) + dt_bias.astype(jnp.float32))
    return g, jax.nn.sigmoid(beta_logit.astype(jnp.float32))


def gated_delta_chunked(q, k, v, g, beta, s0):
    b, n, h, _ = q.shape
    dv = v.shape[-1]
    nc = n // CHUNK

    def chunks(t):
        return t.reshape(b, nc, CHUNK, h, -1).transpose(0, 3, 1, 2, 4)

    q, k, v = chunks(q), chunks(k), chunks(v)
    g = g.reshape(b, nc, CHUNK, h).transpose(0, 3, 1, 2)
    beta = beta.reshape(b, nc, CHUNK, h).transpose(0, 3, 1, 2)
    gc = jnp.cumsum(g, axis=-1)
    incl = jnp.tril(jnp.ones((CHUNK, CHUNK), dtype=bool))
    strict = jnp.tril(jnp.ones((CHUNK, CHUNK), dtype=bool), -1)
    decay = jnp.exp(jnp.where(incl, gc[..., :, None] - gc[..., None, :], -jnp.inf))
    kb = k * beta[..., None]
    m = jnp.where(strict, jnp.einsum('bhnid,bhnjd->bhnij', kb, k) * decay, 0.0)
    rhs = jnp.concatenate([v * beta[..., None], kb * jnp.exp(gc)[..., None]], axis=-1)
    sol = lax.linalg.triangular_solve(m + jnp.eye(CHUNK, dtype=m.dtype), rhs,
                                      left_side=True, lower=True, unit_diagonal=True)
    u, w = sol[..., :dv], sol[..., dv:]
    attn = jnp.where(incl, jnp.einsum('bhnid,bhnjd->bhnij', q, k) * decay, 0.0)
    g_last = gc[..., -1]

    def step(state, xs):
        q_i, k_i, u_i, w_i, a_i, gc_i, gl_i = xs
        v_new = u_i - jnp.einsum('bhck,bhkv->bhcv', w_i, state)
        o_i = (jnp.einsum('bhck,bhkv->bhcv', q_i * jnp.exp(gc_i)[..., None], state)
               + jnp.einsum('bhij,bhjv->bhiv', a_i, v_new))
        k_dec = k_i * jnp.exp(gl_i[..., None] - gc_i)[..., None]
        state = state * jnp.exp(gl_i)[..., None, None] + jnp.einsum('bhck,bhcv->bhkv', k_dec, v_new)
        return state, o_i

    xs = tuple(jnp.moveaxis(t, 2, 0) for t in (q, k, u, w, attn, gc, g_last))
    state, o = lax.scan(step, s0, xs)
    o = o.transpose(1, 0, 3, 2, 4).reshape(b, n, h, dv)
    return o, state


def flip(t):
    return t[:, ::-1]


def dn_gated_out(o, z, norm_w):
    b, n = z.shape[:2]
    o = rmsnorm(o, norm_w) * jax.nn.silu(z.reshape(b, n, DN_HEADS, DN_DV).astype(jnp.float32))
    return o.reshape(b, n, DN_HEADS * DN_DV).astype(z.dtype)


def token_mixer(h, hc, w_in, q_norm_w, kv_norm_w, w_uq, w_ukv, conv_w, a_log, dt_bias,
                dn_norm_w, w_branch_a, w_branch_b, w_out, with_ctx_out):
    b, n, _ = h.shape
    (cq, ckv, kr, qkv, z, a_f, a_b, b_f, b_b, gate_a, gate_b) = jnp.split(h @ w_in, IN_POINTS, axis=-1)
    (cq_c, ckv_c, kr_c, qkv_c, z_c, a_f_c, a_b_c, b_f_c, b_b_c,
     gate_a_c, gate_b_c) = jnp.split(hc @ w_in, IN_POINTS, axis=-1)

    ang_r, ang_c = grid_angles(n)
    qn, qr, kn, v = mla_heads(cq, ckv, q_norm_w, kv_norm_w, w_uq, w_ukv)
    qr = axial_rope(qr, ang_r[:, None, :], ang_c[:, None, :])
    kr = axial_rope(kr, ang_r, ang_c)
    qn_c, qr_c, kn_c, v_c = mla_heads(cq_c, ckv_c, q_norm_w, kv_norm_w, w_uq, w_ukv)
    kn_all = jnp.concatenate([kn_c, kn], axis=1)
    kr_all = jnp.concatenate([kr_c, kr], axis=1)
    v_all = jnp.concatenate([v_c, v], axis=1)
    nb = n // Q_BLOCK

    def to_blocks(t):
        return jnp.swapaxes(t.reshape(b, nb, Q_BLOCK, *t.shape[2:]), 0, 1)

    y_a = lax.map(lambda qs: mla_attend(qs[0], qs[1], kn_all, kr_all, v_all), (to_blocks(qn), to_blocks(qr)))
    y_a = jnp.swapaxes(y_a, 0, 1).reshape(b, n, MLA_HEADS * MLA_V)

    ql, kl, vl = dn_qkv(qkv, conv_w)
    qc, kc, vc = dn_qkv(qkv_c, conv_w)
    g_lf, be_lf = dn_decay(a_f, b_f, a_log[0], dt_bias[0])
    g_lb, be_lb = dn_decay(a_b, b_b, a_log[1], dt_bias[1])
    g_cf, be_cf = dn_decay(a_f_c, b_f_c, a_log[0], dt_bias[0])
    g_cb, be_cb = dn_decay(a_b_c, b_b_c, a_log[1], dt_bias[1])
    s0 = jnp.zeros((b, DN_HEADS, DN_DK, DN_DV), jnp.float32)
    o_cf, s_cf = gated_delta_chunked(qc, kc, vc, g_cf, be_cf, s0)
    o_lf, _ = gated_delta_chunked(ql, kl, vl, g_lf, be_lf, s_cf)
    o_cb, s_cb = gated_delta_chunked(flip(qc), flip(kc), flip(vc), flip(g_cb), flip(be_cb), s0)
    o_lb, _ = gated_delta_chunked(flip(ql), flip(kl), flip(vl), flip(g_lb), flip(be_lb), s_cb)
    y_b = dn_gated_out(o_lf + flip(o_lb), z, dn_norm_w)

    def merge(ya, yb, ga, gb):
        return (jax.nn.sigmoid(ga) * (ya @ w_branch_a) + jax.nn.sigmoid(gb) * (yb @ w_branch_b)) @ w_out

    y = merge(y_a, y_b, gate_a, gate_b)
    if not with_ctx_out:
        return y, None
    y_a_c = mla_attend(qn_c, qr_c, kn_c, kr_c, v_c)
    y_b_c = dn_gated_out(o_cf + flip(o_cb), z_c, dn_norm_w)
    return y, merge(y_a_c, y_b_c, gate_a_c, gate_b_c)


def peer_ffn(h, w_q, sub_keys, u, v):
    b, n, d = h.shape
    nb = (b * n) // PEER_BLOCK
    hb = h.reshape(nb, PEER_BLOCK, d)

    def block(hx):
        t = hx.shape[0]
        q = (hx @ w_q).reshape(t, PEER_HEADS, 2, PEER_QDIM // 2).astype(jnp.float32)
        s = jnp.einsum('thpd,pkd->thpk', q, sub_keys.astype(jnp.float32))
        top_s, top_i = lax.top_k(s, PEER_TOPK)
        cand = top_s[:, :, 0, :, None] + top_s[:, :, 1, None, :]
        cand_idx = top_i[:, :, 0, :, None] * PEER_KEYS + top_i[:, :, 1, None, :]
        best_s, best_pos = lax.top_k(cand.reshape(t, PEER_HEADS, -1), PEER_TOPK)
        idx = jnp.take_along_axis(cand_idx.reshape(t, PEER_HEADS, -1), best_pos, axis=-1)
        wts = jax.nn.softmax(best_s, axis=-1).astype(hx.dtype)
        act = jax.nn.gelu(jnp.einsum('td,thkd->thk', hx, u[idx]), approximate=False)
        return jnp.einsum('thk,thkd->td', wts * act, v[idx])

    return lax.map(block, hb).reshape(b, n, d)


def setup_inputs(seed: int = 0) -> dict:
    key = jax.random.key(seed)
    ks = jax.random.split(key, 26)
    f32 = jnp.float32
    L, D = DEPTH, D_MODEL

    def nrm(k, shape, scale=1.0):
        return jax.random.normal(k, shape, f32) * scale

    dt = jnp.exp(jax.random.uniform(ks[14], (L, 2, DN_HEADS), f32, math.log(1e-3), math.log(1e-1)))
    return {
        'x': nrm(ks[0], (BATCH, SEQ, D)),
        'c': nrm(ks[1], (BATCH, D)),
        'ctx': nrm(ks[2], (BATCH, CTX_LEN, D)),
        'c_ctx': nrm(ks[3], (D,)),
        'w_mod': nrm(ks[4], (L, D, 6 * D), 0.5 * D ** -0.5),
        'b_mod': nrm(ks[5], (L, 6 * D), 0.02),
        'norm1_w': 1.0 + nrm(ks[6], (L, D), 0.1),
        'w_in': nrm(ks[7], (L, D, D_IN), D ** -0.5),
        'mla_q_norm_w': 1.0 + nrm(ks[8], (L, MLA_Q_RANK), 0.1),
        'mla_kv_norm_w': 1.0 + nrm(ks[9], (L, MLA_KV_RANK), 0.1),
        'w_uq': nrm(ks[10], (L, MLA_Q_RANK, MLA_HEADS * (MLA_NOPE + MLA_ROPE)), MLA_Q_RANK ** -0.5),
        'w_ukv': nrm(ks[11], (L, MLA_KV_RANK, MLA_HEADS * (MLA_NOPE + MLA_V)), MLA_KV_RANK ** -0.5),
        'dn_conv_w': nrm(ks[12], (L, CONV_W, DN_QKV), CONV_W ** -0.5),
        'dn_a_log': jnp.log(jax.random.uniform(ks[13], (L, 2, DN_HEADS), f32, 1.0, 16.0)),
        'dn_dt_bias': dt + jnp.log(-jnp.expm1(-dt)),
        'dn_norm_w': 1.0 + nrm(ks[15], (L, DN_DV), 0.1),
        'w_branch_a': nrm(ks[16], (L, MLA_HEADS * MLA_V, D), (MLA_HEADS * MLA_V) ** -0.5),
        'w_branch_b': nrm(ks[17], (L, DN_HEADS * DN_DV, D), (DN_HEADS * DN_DV) ** -0.5),
        'w_out': nrm(ks[18], (L, D, D), D ** -0.5),
        'norm2_w': 1.0 + nrm(ks[19], (L, D), 0.1),
        'peer_w_q': nrm(ks[20], (L, D, PEER_HEADS * PEER_QDIM), D ** -0.5),
        'peer_sub_keys': nrm(ks[21], (L, 2, PEER_KEYS, PEER_QDIM // 2), (PEER_QDIM // 2) ** -0.5),
        'peer_u': nrm(ks[22], (L, PEER_EXPERTS, D), D ** -0.5),
        'peer_v': nrm(ks[23], (L, PEER_EXPERTS, D), PEER_HEADS ** -0.5),
        'final_norm_w': 1.0 + nrm(ks[24], (D,), 0.1),
    }


def reference(x, c, ctx, c_ctx, w_mod, b_mod, norm1_w, w_in, mla_q_norm_w, mla_kv_norm_w,
              w_uq, w_ukv, dn_conv_w, dn_a_log, dn_dt_bias, dn_norm_w, w_branch_a, w_branch_b,
              w_out, norm2_w, peer_w_q, peer_sub_keys, peer_u, peer_v, final_norm_w):
    silu_c = jax.nn.silu(c)
    silu_cc = jax.nn.silu(c_ctx)
    for i in range(DEPTH):
        last = i == DEPTH - 1
        mod = (silu_c @ w_mod[i] + b_mod[i])[:, None, :]
        sh1, sc1, g1, sh2, sc2, g2 = jnp.split(mod, 6, axis=-1)
        mod_c = silu_cc @ w_mod[i] + b_mod[i]
        shc1, scc1, gc1, shc2, scc2, gc2 = jnp.split(mod_c, 6, axis=-1)
        h = modulate(x, norm1_w[i], sh1, sc1)
        hc = modulate(ctx, norm1_w[i], shc1, scc1)
        y, yc = token_mixer(h, hc, w_in[i], mla_q_norm_w[i], mla_kv_norm_w[i], w_uq[i], w_ukv[i],
                            dn_conv_w[i], dn_a_log[i], dn_dt_bias[i], dn_norm_w[i],
                            w_branch_a[i], w_branch_b[i], w_out[i], not last)
        x = x + g1 * y
        x = x + g2 * peer_ffn(modulate(x, norm2_w[i], sh2, sc2), peer_w_q[i], peer_sub_keys[i], peer_u[i], peer_v[i])
        if not last:
            ctx = ctx + gc1 * yc
            ctx = ctx + gc2 * peer_ffn(modulate(ctx, norm2_w[i], shc2, scc2), peer_w_q[i], peer_sub_keys[i], peer_u[i], peer_v[i])
    return rmsnorm(x, final_norm_w)
```

```python
import numpy as np
import concourse.bass as bass
import concourse.mybir as mybir
from concourse.bass_utils import run_bass_kernel_spmd
from contextlib import ExitStack

F32 = mybir.dt.float32
BF16 = mybir.dt.bfloat16
U32 = mybir.dt.uint32
I32 = mybir.dt.int32
AF = mybir.ActivationFunctionType
ALU = mybir.AluOpType

ENGS = ['pe', 'dve', 'act', 'pool', 'sp']
EPOCH = 20000
NSLOT = 6
SKIP_SELF = False


class Buf:
    __slots__ = ('w', 'r', 'name')

    def __init__(self, name=''):
        self.w = None
        self.r = {}
        self.name = name


class Ctx:
    def __init__(self, nc, stack):
        self.nc = nc
        self.stack = stack
        self.lists = {e: [] for e in ENGS}
        self.sems = {}
        self.cnt = {}
        self.epoch = {e: 0 for e in ENGS}
        self.seen = {e: {} for e in ENGS}
        self.dslot = {e: 0 for e in ENGS}
        self.dep = {e: 0 for e in ENGS}
        self.nsem = 0

    def _sem(self, key):
        if key not in self.sems:
            self.sems[key] = self.stack.enter_context(self.nc.semaphore(f"s{self.nsem}"))
            self.nsem += 1
            self.cnt[key] = 0
        return self.sems[key]

    def _issue(self, eng, fn, r, w, dma=False, skip_self=False, dinc=16):
        deps = {}

        def add(ev):
            if ev is None:
                return
            k, v = ev
            if deps.get(k, 0) < v:
                deps[k] = v
        for b in r:
            add(b.w)
        for b in w:
            add(b.w)
            for k, v in b.r.items():
                add((k, v))
        if dma:
            s = self.dslot[eng]
            self.dslot[eng] = (s + 1) % NSLOT
            key = ('d', eng, s, self.dep.get((eng, s), 0))
            self._sem(key)
            if self.cnt[key] + dinc > EPOCH:
                self.dep[(eng, s)] = self.dep.get((eng, s), 0) + 1
                old = key
                key = ('d', eng, s, self.dep[(eng, s)])
                self._sem(key)
                add((old, self.cnt[old]))
            else:
                add((key, self.cnt[key]))
            inc = dinc
        else:
            key = ('c', eng, self.epoch[eng])
            self._sem(key)
            if self.cnt[key] + 1 > EPOCH:
                self.epoch[eng] += 1
                key = ('c', eng, self.epoch[eng])
                self._sem(key)
            inc = 1
        waits = []
        seen = self.seen[eng]
        for k, v in deps.items():
            if v <= 0:
                continue
            if skip_self and k[0] == 'c' and k[1] == eng:
                continue
            if seen.get(k, 0) < v:
                seen[k] = v
                waits.append((k, v))
        self.cnt[key] += inc
        ev = (key, self.cnt[key])
        for b in r:
            if b.r.get(key, 0) < ev[1]:
                b.r[key] = ev[1]
        for b in w:
            b.w = ev
            b.r = {}
        self.lists[eng].append((waits, fn, key, inc))
        return ev

    def mm(self, fn, r, w):
        return self._issue('pe', fn, r, w, skip_self=True)

    def dve(self, fn, r, w):
        return self._issue('dve', fn, r, w, skip_self=SKIP_SELF)

    def act(self, fn, r, w):
        return self._issue('act', fn, r, w, skip_self=SKIP_SELF)

    def pool(self, fn, r, w):
        return self._issue('pool', fn, r, w)

    def dma(self, fn, r, w, q='sp'):
        return self._issue(q, fn, r, w, dma=True)

    def coll(self, fn, r, w):
        return self._issue('pool', fn, r, w, dma=True, dinc=1)

    def wait_all(self, eng, bufs):
        deps = {}
        for b in bufs:
            if b.w is not None:
                k, v = b.w
                deps[k] = max(deps.get(k, 0), v)
        self.lists[eng].append(([(k, v) for k, v in deps.items()], None, None, 0))

    def emit(self):
        nc = self.nc
        sems = self.sems
        lists = self.lists
        with nc.Block() as block:
            def run(e, name):
                for waits, fn, key, inc in lists[name]:
                    for k, v in waits:
                        e.wait_ge(sems[k], v)
                    if fn is not None:
                        ins = fn(e)
                        ins.then_inc(sems[key], inc)

            @block.tensor
            def _(e):
                run(e, 'pe')

            @block.vector
            def _(e):
                run(e, 'dve')

            @block.scalar
            def _(e):
                run(e, 'act')

            @block.gpsimd
            def _(e):
                run(e, 'pool')

            @block.sync
            def _(e):
                run(e, 'sp')
        self.lists = {e: [] for e in ENGS}

    def MM(self, out, lhsT, rhs, r, w, start=True, stop=True):
        return self.mm(lambda e: e.matmul(out, lhsT=lhsT, rhs=rhs, start=start, stop=stop), r, w)

    def TR(self, out, in_, ident, r, w):
        return self.mm(lambda e: e.transpose(out, in_, ident), r, w)

    def ACT(self, out, in_, func, r, w, **kw):
        return self.act(lambda e: e.activation(out=out, in_=in_, func=func, **kw), r, w)

    def _ve(self, eng):
        return {'dve': self.dve, 'pool': self.pool}[eng]

    def TT(self, out, in0, in1, op, r, w, eng='dve'):
        return self._ve(eng)(lambda e: e.tensor_tensor(out=out, in0=in0, in1=in1, op=op), r, w)

    def TS(self, out, in0, s1, s2, op0, op1, r, w, eng='dve'):
        if op1 is None:
            return self._ve(eng)(lambda e: e.tensor_scalar(out=out, in0=in0, scalar1=s1, scalar2=None, op0=op0), r, w)
        return self._ve(eng)(lambda e: e.tensor_scalar(out=out, in0=in0, scalar1=s1, scalar2=s2, op0=op0, op1=op1), r, w)

    def STT(self, out, in0, scalar, in1, op0, op1, r, w):
        return self.dve(lambda e: e.scalar_tensor_tensor(out=out, in0=in0, scalar=scalar, in1=in1, op0=op0, op1=op1), r, w)

    def CP(self, out, in_, r, w, eng='dve'):
        if eng == 'act':
            return self.act(lambda e: e.activation(out=out, in_=in_, func=AF.Copy), r, w)
        return self._ve(eng)(lambda e: e.tensor_copy(out=out, in_=in_), r, w)

    def RCP(self, out, in_, r, w):
        return self.dve(lambda e: e.reciprocal(out=out, in_=in_), r, w)

    def MSET(self, out, val, r, w, eng='dve'):
        return self._ve(eng)(lambda e: e.memset(out, val), r, w)

    def DMA(self, out, in_, r, w, q='sp'):
        return self.dma(lambda e: e.dma_start(out=out, in_=in_), r, w, q=q)


D = 2048
NTOK = 2112
BLKS = [(0, 64, 1)] + [(64 + 512 * i, 512, 0) for i in range(4)]
DIN = 9056
MCH = 71
NT = 8448
NTL = 66
NQ = 2048
SCALE = 192 ** -0.5
TB = 256
NBLK = NQ // TB
NEG = -1.0e30
GR = 27 * 128
RG = [[0, 1, 2, 3], [4, 5, 6, 7]]
BLK = [(512 * i, 512) for i in range(16)] + [(8192, 256)]
H_AB = 832 + 3072 + 1024


def pieces(t0, nb):
    out = []
    t = t0
    while t < t0 + nb:
        if t < 256:
            s, c = t // 64, t % 64
            ln = min(64 - c, t0 + nb - t)
        else:
            u = t - 256
            s, c = u // 2048, 64 + u % 2048
            ln = min(2048 - u % 2048, t0 + nb - t)
        out.append((t - t0, s, c, ln))
        t += ln
    return out


def phase_A(nc, k, T, stop_after=None):
    I = T['I']
    x_own = I("x_own", [NTOK, D]); cc = I("cc", [128, 16, 2]); w_mod = I("w_mod", [96, 128, 16, 128]); b_mod = I("b_mod", [128, 96])
    n1w = I("n1w", [128, 16]); w_in = I("w_in", [MCH, 128, 16, 128])
    XT, HW, MOD, G1i, G1fi = T['XT'], T['HW'], T['MOD'], T['G1i'], T['G1fi']
    with ExitStack() as st:
        sb = lambda n, s, dt=F32: st.enter_context(nc.sbuf_tensor("a_" + n, s, dt))
        ps = lambda n, s, dt=F32: st.enter_context(nc.psum_tensor("a_" + n, s, dt))
        b_c = Buf()
        idf = sb("idf", [128, 128]); oht = sb("oht", [128, 4])
        k.DMA(idf[:], T['ident'], [], [b_c]); k.DMA(oht[:], T['oh4'], [], [b_c])
        xtok = [sb(f"xtok{i}", [128, D]) for i in range(2)]; b_xtok = [Buf(), Buf()]
        xTs = [sb(f"xTs{i}", [128, 16, 128]) for i in range(2)]; b_xTs = [Buf(), Buf()]
        pT = [ps(f"pT{i}", [128, 4, 128]) for i in range(2)]; b_pT = [Buf(), Buf()]
        XTv = XT.rearrange("(c p) t -> p c t", p=128)
        tiles = [(0, 64)] + [(64 + 128 * i, 128) for i in range(16)]
        for ti, (r0, np_) in enumerate(tiles):
            i = ti % 2
            k.DMA(xtok[i][0:np_, :], x_own[r0:r0 + np_, :], [], [b_xtok[i]], q='sp' if i == 0 else 'pool')
            for c in range(16):
                pb = (c // 4) % 2
                k.TR(pT[pb][:, c % 4, 0:np_], xtok[i][0:np_, c * 128:(c + 1) * 128], idf[0:np_, 0:np_], [b_xtok[i], b_c], [b_pT[pb]])
                if c % 4 == 3:
                    k.CP(xTs[i][:, c - 3:c + 1, 0:np_], pT[pb][:, :, 0:np_], [b_pT[pb]], [b_xTs[i]], eng='act' if pb == 0 else 'dve')
            k.DMA(XTv[:, :, r0:r0 + np_], xTs[i][:, :, 0:np_], [b_xTs[i]], [T['b_XT']])
        cct = sb("cct", [128, 16, 2]); b_cct = Buf()
        bmt = sb("bmt", [128, 96]); b_bmt = Buf()
        n1t = sb("n1t", [128, 16]); b_n1t = Buf()
        modt = sb("modt", [128, 96, 2]); b_modt = Buf()
        scl = sb("scl", [128, 16, 2]); b_scl = Buf()
        ones = sb("ones", [128, 128]); b_ones = Buf()
        wst = [sb(f"wst{i}", [128, 16, 128]) for i in range(2)]; b_wst = [Buf(), Buf()]
        wbf = [sb(f"wbf{i}", [128, 16, 128], BF16) for i in range(2)]; b_wbf = [Buf(), Buf()]
        hT = sb("hT", [128, 16, NTOK], BF16); b_hT = Buf()
        xt = sb("xt", [128, 16, 512]); b_xt = Buf()
        sq = [sb(f"sq{i}", [128, 512]) for i in range(2)]; b_sq = [Buf(), Buf()]
        rstd = sb("rstd", [128, 512]); b_rstd = Buf()
        ot = [sb(f"ot{i}", [128, NTOK]) for i in range(2)]; b_ot = [Buf(), Buf()]
        ex = [sb(f"ex{i}", [128, NTOK], BF16) for i in range(2)]; b_ex = [Buf(), Buf()]
        exf = sb("exf", [128, NTOK]); b_exf = Buf()
        pmod = ps("pmod", [128, 512]); b_pmod = Buf()
        pg = [ps(f"pg{i}", [128, 512]) for i in range(5)]; b_pg = [Buf() for _ in range(5)]
        pss = pg[0]; b_pss = b_pg[0]
        k.DMA(cct[:], cc, [], [b_cct]); k.DMA(bmt[:], b_mod, [], [b_bmt]); k.DMA(n1t[:], n1w, [], [b_n1t])
        k.MSET(ones[:], 1.0, [], [b_ones])
        k.ACT(cct[:], cct[:], AF.Silu, [], [b_cct])
        for j in range(96):
            i = j % 2
            k.DMA(wst[i][:], w_mod[j], [], [b_wst[i]], q='sp' if j % 2 == 0 else 'pool')
            for c in range(16):
                k.MM(pmod[:, j * 2:j * 2 + 2], wst[i][:, c, :], cct[:, c, :], [b_wst[i], b_cct], [b_pmod], start=(c == 0), stop=(c == 15))
            k.TS(modt[:, j, :], pmod[:, j * 2:j * 2 + 2], bmt[:, j:j + 1], None, ALU.add, None, [b_pmod, b_bmt], [b_modt])
        modS = sb("modS", [128, 2, 96])
        for s_ in range(2):
            k.CP(modS[:, s_, :], modt[:, :, s_], [b_modt], [b_modt])
        k.DMA(MOD, modS[:], [b_modt], [T['b_MOD']])
        for s in range(2):
            k.STT(scl[:, :, s], modt[:, 16:32, s], 1.0, n1t[:], ALU.add, ALU.mult, [b_modt, b_n1t], [b_scl])
        for (t0, nb, s) in BLKS:
            k.DMA(xt[:, :, 0:nb], XTv[:, :, t0:t0 + nb], [T['b_XT']], [b_xt])
            for c in range(16):
                i = c % 2
                k.ACT(sq[i][:, 0:nb], xt[:, c, 0:nb], AF.Square, [b_xt], [b_sq[i]])
                k.MM(pss[:, 0:nb], ones[:], sq[i][:, 0:nb], [b_sq[i], b_ones], [b_pss], start=(c == 0), stop=(c == 15))
            k.TS(rstd[:, 0:nb], pss[:, 0:nb], 1.0 / D, 1e-6, ALU.mult, ALU.add, [b_pss], [b_rstd])
            k.ACT(rstd[:, 0:nb], rstd[:, 0:nb], AF.Sqrt, [], [b_rstd])
            k.RCP(rstd[:, 0:nb], rstd[:, 0:nb], [], [b_rstd])
            for c in range(16):
                i = c % 2
                k.TT(sq[i][:, 0:nb], xt[:, c, 0:nb], rstd[:, 0:nb], ALU.mult, [b_xt, b_rstd], [b_sq[i]])
                k.ACT(hT[:, c, t0:t0 + nb], sq[i][:, 0:nb], AF.Identity, [b_sq[i], b_scl, b_modt], [b_hT],
                      scale=scl[:, c, s:s + 1], bias=modt[:, c, s:s + 1])
        nex = 0
        for m in range(MCH):
            i = m % 2
            mw = min(128, DIN - m * 128)
            k.DMA(wbf[i][:], w_in[m], [], [b_wbf[i]], q='pool')
            for bi, (t0, nb, s) in enumerate(BLKS):
                for c in range(16):
                    k.MM(pg[bi][0:mw, 0:nb], wbf[i][:, c, 0:mw], hT[:, c, t0:t0 + nb], [b_wbf[i], b_hT], [b_pg[bi]],
                         start=(c == 0), stop=(c == 15))
                k.CP(ot[i][0:mw, t0:t0 + nb], pg[bi][0:mw, 0:nb], [b_pg[bi]], [b_ot[i]], eng='act' if bi % 2 == 0 else 'dve')
            k.DMA(HW[m * 128:m * 128 + mw, :], ot[i][0:mw, :], [b_ot[i]], [T['b_HW']])
            if 4 <= m <= 30:
                for s in range(4):
                    e_ = nex % 2; nex += 1
                    k.ACT(ex[e_][:], ot[i][:], AF.Identity, [b_ot[i], b_c], [b_ex[e_]], scale=oht[:, s:s + 1])
                    k.DMA(G1i[s * GR + (m - 4) * 128: s * GR + (m - 3) * 128, :], ex[e_][:], [b_ex[e_]], [T['b_G1i']],
                          q='sp' if e_ == 0 else 'pool')
            if m == 38:
                for s in range(4):
                    k.ACT(exf[64:96, :], ot[i][64:96, :], AF.Identity, [b_ot[i], b_c], [b_exf], scale=oht[64:96, s:s + 1])
                    k.DMA(G1fi[s * 32:(s + 1) * 32, :], exf[64:96, :], [b_exf], [T['b_G1fi']])
        k.wait_all('sp', [T['b_HW'], T['b_G1i'], T['b_G1fi'], T['b_MOD'], T['b_XT']])
        k.emit()


def allreduce_chunks(k, src, dst, rows, ch, r, w):
    for r0 in range(0, rows, ch):
        k.coll(lambda e, r0=r0: e.collective_compute("AllReduce", ALU.add, replica_groups=RG, ins=[src[r0:r0 + ch, :].opt()],
                                                     outs=[dst[r0:r0 + ch, :].opt()]), r, w)


def rope_tables():
    n = 8192
    rows = np.repeat(np.arange(n // 64, dtype=np.float32), 64)
    cols = np.tile(np.arange(64, dtype=np.float32), n // 64)
    inv = (np.float32(10000.0) ** (-np.arange(0, 32, 2, dtype=np.float32) / np.float32(32))).astype(np.float32)
    ar = rows[:, None] * inv; ac = cols[:, None] * inv
    cos = np.concatenate([np.cos(ar), np.cos(ar), np.cos(ac), np.cos(ac)], 1).T
    sin = np.concatenate([-np.sin(ar), np.sin(ar), -np.sin(ac), np.sin(ac)], 1).T
    return cos.astype(np.float32), sin.astype(np.float32)


PERM = np.concatenate([np.arange(16, 32), np.arange(0, 16), np.arange(48, 64), np.arange(32, 48)])


def gdn_consts():
    m = np.arange(128)[:, None]; t = np.arange(128)[None, :]
    f = lambda c: c.astype(np.float32)
    fw_ = [f(m <= t), f(m > t), -30000.0 * f(t > m), f(t < m)]
    bw_ = [f(m >= t), f(m < t), -30000.0 * f(t < m), f(t > m)]
    bm = []
    for lower in (True, False):
        for l in range(7):
            sz = 2 ** l
            same = ((m // (2 * sz)) == (t // (2 * sz))) & ((m // sz) != (t // sz))
            bm.append(f(same & ((m > t) if lower else (m < t))))
    return np.ascontiguousarray(np.stack(fw_ + bw_, 1)), np.eye(128, dtype=np.float32), np.ascontiguousarray(np.stack(bm, 1))


class StopBuild(Exception):
    pass


def phase_B(nc, k, T, do1=True, do2=True, stage=99):
    I = T['I']
    COS = I("COS", [64, NT]); SIN = I("SIN", [64, NT]); kvw = I("kvw", [128, 2]); wukv = I("wukv", [256, 512])
    convw = I("convw", [128, 6, 5]); alog = I("alog", [128, 4]); dtb = I("dtb", [128, 4]); masks = I("masks", [128, 8, 128])
    bmask = I("bmask", [128, 14, 128]); ident = T['ident']
    G1o, G1fo, GKi, GVi, GOi, KR = T['G1o'], T['G1fo'], T['GKi'], T['GVi'], T['GOi'], T['KR']
    G1v = G1o.rearrange("(s c p) t -> s p c t", s=4, p=128)
    bG1l = T['b_G1o']

    def g1b(r0, r1):
        return [bG1l[c_] for c_ in range(r0 // 512, (r1 - 1) // 512 + 1)]
    if True:
        b_out = Buf()
        with ExitStack() as st:
          if do1:
            sb = lambda n, s, dt=F32: st.enter_context(nc.sbuf_tensor("b_" + n, s, dt))
            ps = lambda n, s, dt=F32: st.enter_context(nc.psum_tensor("b_" + n, s, dt))
            ones = sb("ones1", [128, 128]); b_c = Buf()
            kvwt = sb("kvwt", [128, 2]); wst = sb("wukvs", [128, 2, 512]); wbf = sb("wukvb", [128, 2, 512], BF16)
            ckvn = sb("ckvn", [128, 2, NT], BF16); b_ckvn = Buf()
            xt = [sb(f"ckx{i}", [128, 2, 512], BF16) for i in range(2)]; b_xt = [Buf(), Buf()]
            sq = sb("cksq", [128, 512]); b_sq = Buf()
            rstd = sb("ckr", [128, 512]); b_rstd = Buf()
            kst = [sb(f"kst{i}", [128, 512], BF16) for i in range(2)]; b_kst = [Buf(), Buf()]
            vst = [sb(f"vst{i}", [128, 128], BF16) for i in range(2)]; b_vst = [Buf(), Buf()]
            rp = [sb(f"rp{i}", [64, 4, 512]) for i in range(2)]; b_rp = [Buf(), Buf()]
            rpo = [sb(f"rpo{i}", [64, 512], BF16) for i in range(2)]
            rpb = [sb(f"rpb{i}", [64, 2, 512], BF16) for i in range(2)]
            oht = sb("oht1", [128, 4]); k.DMA(oht[:], T["oh4"], [], [b_c]); nk = [0]; b_rpo = [Buf(), Buf()]
            pss = ps("pss1", [128, 512]); b_pss = Buf()
            pk = [ps(f"pk{i}", [128, 512]) for i in range(2)]; b_pk = [Buf(), Buf()]
            pvt = [ps(f"pvt{i}", [128, 4, 128]) for i in range(2)]; pv = [pvt[0][:, 0, :], pvt[1][:, 0, :]]; b_pv = [Buf(), Buf()]
            k.MSET(ones[:], 1.0, [], [b_c])
            k.DMA(kvwt[:], kvw, [], [b_c])
            k.DMA(wst[:], wukv.rearrange("(c p) n -> p c n", p=128), [], [b_c])
            k.CP(wbf[:], wst[:], [b_c], [b_c])
            for bi, (t0, nb) in enumerate(BLK):
                i = bi % 2
                for (off, s_, c0, ln) in pieces(t0, nb):
                    k.DMA(xt[i][:, :, off:off + ln], G1v[s_][:, 0:2, c0:c0 + ln], g1b(s_ * GR, s_ * GR + 256), [b_xt[i]])
                for c in range(2):
                    k.ACT(sq[:, 0:nb], xt[i][:, c, 0:nb], AF.Square, [b_xt[i]], [b_sq])
                    k.MM(pss[:, 0:nb], ones[:], sq[:, 0:nb], [b_sq, b_c], [b_pss], start=(c == 0), stop=(c == 1))
                k.TS(rstd[:, 0:nb], pss[:, 0:nb], 1.0 / 256, 1e-6, ALU.mult, ALU.add, [b_pss], [b_rstd])
                k.ACT(rstd[:, 0:nb], rstd[:, 0:nb], AF.Sqrt, [], [b_rstd])
                k.RCP(rstd[:, 0:nb], rstd[:, 0:nb], [], [b_rstd])
                for c in range(2):
                    k.STT(ckvn[:, c, t0:t0 + nb], xt[i][:, c, 0:nb], kvwt[:, c:c + 1], rstd[:, 0:nb], ALU.mult, ALU.mult,
                          [b_xt[i], b_rstd, b_c], [b_ckvn])
                for hl in range(2):
                    for c in range(2):
                        k.MM(pk[hl][:, 0:nb], wbf[:, c, hl * 256:hl * 256 + 128], ckvn[:, c, t0:t0 + nb], [b_c, b_ckvn], [b_pk[hl]],
                             start=(c == 0), stop=(c == 1))
                    for s_ in range(4):
                        e_ = nk[0] % 2; nk[0] += 1
                        k.ACT(kst[e_][:, 0:nb], pk[hl][:, 0:nb], AF.Identity, [b_pk[hl], b_c], [b_kst[e_]], scale=oht[:, s_:s_ + 1])
                        k.DMA(GKi[s_ * 256 + hl * 128:s_ * 256 + (hl + 1) * 128, t0:t0 + nb], kst[e_][:, 0:nb], [b_kst[e_]], [T['b_GKi']],
                              q='pool')
                for tt in range(nb // 128):
                    ti = t0 // 128 + tt
                    for hl in range(2):
                        for c in range(2):
                            k.MM(pv[hl], ckvn[:, c, ti * 128:(ti + 1) * 128], wbf[:, c, hl * 256 + 128:hl * 256 + 256],
                                 [b_c, b_ckvn], [b_pv[hl]], start=(c == 0), stop=(c == 1))
                        for s_ in range(4):
                            e_ = nk[0] % 2; nk[0] += 1
                            k.TS(vst[e_][:], pv[hl], oht[:, s_:s_ + 1], None, ALU.mult, None, [b_pv[hl], b_c], [b_vst[e_]])
                            r0_ = ((s_ * 2 + hl) * NTL + ti) * 128
                            k.DMA(GVi[r0_:r0_ + 128, :], vst[e_][:], [b_vst[e_]], [T['b_GVi']], q='pool')
                for (off, s_, c0, ln) in pieces(t0, nb):
                    k.DMA(rpb[i][:, 0, off:off + ln], G1o[s_ * GR + 256:s_ * GR + 320, c0:c0 + ln], g1b(s_ * GR + 256, s_ * GR + 320), [b_rp[i]])
                    for (d0, s0) in ((0, 16), (16, 0), (32, 48), (48, 32)):
                        k.DMA(rpb[i][d0:d0 + 16, 1, off:off + ln], G1o[s_ * GR + 256 + s0:s_ * GR + 256 + s0 + 16, c0:c0 + ln], g1b(s_ * GR + 256, s_ * GR + 320), [b_rp[i]],
                              q='pool')
                k.DMA(rp[i][:, 2, 0:nb], COS[:, t0:t0 + nb], [], [b_rp[i]])
                k.DMA(rp[i][:, 3, 0:nb], SIN[:, t0:t0 + nb], [], [b_rp[i]])
                k.TT(rp[i][:, 0, 0:nb], rpb[i][:, 0, 0:nb], rp[i][:, 2, 0:nb], ALU.mult, [], [b_rp[i]])
                k.TT(rp[i][:, 1, 0:nb], rpb[i][:, 1, 0:nb], rp[i][:, 3, 0:nb], ALU.mult, [], [b_rp[i]])
                k.TT(rpo[i][:, 0:nb], rp[i][:, 0, 0:nb], rp[i][:, 1, 0:nb], ALU.add, [b_rp[i]], [b_rpo[i]], eng='pool')
                k.DMA(KR[:, t0:t0 + nb], rpo[i][:, 0:nb], [b_rpo[i]], [T['b_KR']])
            k.wait_all('sp', [T['b_KR'], T['b_GKi'], T['b_GVi']])
            k.emit()
        with ExitStack() as st:
          if do2:
            sb = lambda n, s, dt=F32: st.enter_context(nc.sbuf_tensor("b_" + n, s, dt))
            ps = lambda n, s, dt=F32: st.enter_context(nc.psum_tensor("b_" + n, s, dt))
            b_c = Buf()
            ones = sb("ones2", [128, 128]); onesb = sb("ones2b", [128, 128], BF16)
            idf = sb("idf", [128, 128]); idb = sb("idb", [128, 128], BF16)
            mk = sb("mk", [128, 8, 128]); abt = sb("abt", [128, NTL, 8]); cwt = sb("cwt", [128, 6, 5])
            bmk = sb("bmk", [128, 14, 128]); II32 = sb("II32", [128, 2, 128]); IIb = sb("IIb", [128, 2, 128], BF16)
            alt = sb("alt", [128, 4]); dtt = sb("dtt", [128, 4]); oht = sb("oht2", [128, 4])
            k.MSET(ones[:], 1.0, [], [b_c]); k.MSET(onesb[:], 1.0, [], [b_c])
            for dst, src in [(idf, ident), (mk, masks), (cwt, convw), (alt, alog), (dtt, dtb), (bmk, bmask), (oht, T['oh4'])]:
                k.DMA(dst[:], src, [], [b_c])
            k.CP(idb[:], idf[:], [b_c], [b_c])
            for q_ in range(2):
                k.CP(II32[:, q_, :], idf[:], [b_c], [b_c]); k.CP(IIb[:, q_, :], idf[:], [b_c], [b_c])
            k.ACT(alt[:], alt[:], AF.Exp, [b_c], [b_c])
            pre = sb("pre", [128, NT]); b_pre = Buf()
            cvb = sb("cvb", [128, NT]); b_cv = Buf()
            qkb = [sb(f"qkb{g}", [128, NT], BF16) for g in range(3)]; b_qk = [Buf() for _ in range(3)]
            sqb = sb("sqb", [128, 512], BF16); b_sqb = Buf()
            rs2 = sb("rs2", [128, 512]); b_rs2 = Buf()
            pss = ps("pss2", [128, 512]); b_pss = Buf()
            gt = sb("gt", [128, NTL]); bet = sb("bet", [128, NTL]); egc = sb("egc", [128, NTL]); erest = sb("erest", [128, NTL])
            egl = sb("egl", [128, NTL]); nc1 = sb("nc1", [128, NTL]); b_sc = Buf()
            psc = ps("psc", [128, 4, 128]); b_psc = Buf()
            b_Tg = Buf()
            b_Dm = Buf()
            b_MA = Buf()
            b_TR = Buf()
            b_Cst = Buf()
            b_DE = Buf(); b_DEb = Buf()
            b_Gb = Buf()
            b_bk = Buf()
            rr = sb("rr", [128, 128], BF16); b_rr = Buf()
            oq = sb("oq", [128, 128]); b_oq = Buf()
            vn = sb("vn", [128, 128], BF16); b_vn = Buf()
            S = sb("S", [128, 128]); Sb = sb("Sb", [128, 128], BF16); b_S = Buf(); b_S32 = Buf()
            ost = [sb(f"ost{i}", [128, 128]) for i in range(2)]; b_ost = [Buf(), Buf()]
            psA = ps("psA", [128, 4, 128]); b_psA = Buf()
            psT = ps("psT", [128, 8, 128], BF16); b_psT = Buf()
            psB = ps("psB", [128, 4, 128]); b_psB = Buf()
            psS = ps("psS", [128, 4, 128]); b_psS = Buf()
            psUt = ps("psU", [128, 4, 128]); psU = psUt[:, 0, :]; b_psU = Buf()
            psO = ps("psO", [128, 4, 128]); b_psO = Buf()
            cand = [sb(f"cand{i}", [128, NT], BF16) for i in range(2)]; b_cand = [Buf(), Buf()]
            oex = [sb(f"oex{i}", [128, 128], BF16) for i in range(2)]; b_oex = [Buf(), Buf()]
            nq = [0]
            ab8 = pre[0:8, :]; abc = cvb[0:8, :]; b_ab8 = b_pre; b_abc = b_cv
            G1fv = G1fo.rearrange("(s k h) t -> s k h t", s=4, k=4)
            for jc in range(4):
                for (off, s_, c0, ln) in pieces(0, NT):
                    for kind in range(4):
                        k.DMA(abc[kind * 2:kind * 2 + 2, off:off + ln], G1fv[s_][kind, 2 * jc:2 * jc + 2, c0:c0 + ln], [T['b_G1fo']], [b_abc],
                              q='sp' if kind % 2 == 0 else 'pool')
                if jc == 0:
                    k.TS(ab8, abc, oht[0:8, 0:1], None, ALU.mult, None, [b_abc, b_c], [b_ab8])
                else:
                    k.STT(ab8, abc, oht[0:8, jc:jc + 1], ab8, ALU.mult, ALU.add, [b_abc, b_c], [b_ab8])
            for half in range(2):
                for tq in range(33):
                    ti = half * 33 + tq
                    k.TR(psO[:, :, :].rearrange("p a b -> p (a b)")[:, tq * 8:tq * 8 + 8], ab8[0:8, ti * 128:(ti + 1) * 128], idf[0:8, 0:8],
                         [b_ab8, b_c], [b_psO])
                k.CP(abt[:, half * 33:(half + 1) * 33, :], psO[:, :, :].rearrange("p a b -> p (a b)")[:, 0:264].rearrange("p (a b) -> p a b", b=8),
                     [b_psO], [b_c])
            oacc = pre[:, 0:8192].rearrange("p (a b) -> p a b", b=128)
            psA2 = pss[:].rearrange("p (a b) -> p a b", b=128)
            psB2 = psc[:].rearrange("p (a b) c -> p a b c", b=2)
            Tg2 = sb("Tg2", [128, 2, 128]); Dm2 = sb("Dm2", [128, 2, 128]); DmS2 = sb("DmS2", [128, 2, 128])
            Mm2 = sb("Mm2", [128, 2, 128], BF16); At2 = sb("At2", [128, 2, 128], BF16); TRt2 = sb("TRt2", [128, 8, 128], BF16)
            Cst2 = sb("Cst2", [128, 2, 7, 128], BF16); DE32_2 = sb("DE32_2", [128, 2, 2, 128]); DEb2 = sb("DEb2", [128, 2, 2, 128], BF16)
            Gb2 = sb("Gb2", [128, 2, 128], BF16); bv2 = sb("bv2", [128, 2, 128]); kdec2 = sb("kdec2", [128, 2, 128], BF16)
            II32_2 = sb("II32_2", [128, 2, 2, 128]); IIb_2 = sb("IIb_2", [128, 2, 2, 128], BF16)
            for a_ in range(2):
                for q_ in range(2):
                    k.CP(II32_2[:, a_, q_, :], idf[:], [b_c], [b_c]); k.CP(IIb_2[:, a_, q_, :], idf[:], [b_c], [b_c])
            try:
             for hl in range(2):
                for g in range(3):
                    for jc in range(4):
                        ci = nq[0] % 2; nq[0] += 1
                        rb = 320 + g * 1024 + (2 * jc + hl) * 128
                        for pi_, (off, s_, c0, ln) in enumerate(pieces(0, NT)):
                            k.DMA(cand[ci][:, off:off + ln], G1o[s_ * GR + rb:s_ * GR + rb + 128, c0:c0 + ln], g1b(s_ * GR + rb, s_ * GR + rb + 128), [b_cand[ci]],
                                  q='sp' if pi_ % 2 == 0 else 'pool')
                        if jc == 0:
                            k.TS(pre[:], cand[ci][:], oht[:, 0:1], None, ALU.mult, None, [b_cand[ci], b_c], [b_pre])
                        else:
                            k.STT(pre[:], cand[ci][:], oht[:, jc:jc + 1], pre[:], ALU.mult, ALU.add, [b_cand[ci], b_c], [b_pre])
                    for (lo, hi) in [(0, 256), (256, NT)]:
                        k.TS(cvb[:, lo:hi], pre[:, lo:hi], cwt[:, hl * 3 + g, 2:3], None, ALU.mult, None, [b_pre, b_c], [b_cv])
                        for s_ in (-2, -1, 1, 2):
                            a_, b_ = (lo - s_, hi) if s_ < 0 else (lo, hi - s_)
                            k.STT(cvb[:, a_:b_], pre[:, a_ + s_:b_ + s_], cwt[:, hl * 3 + g, s_ + 2:s_ + 3], cvb[:, a_:b_],
                                  ALU.mult, ALU.add, [b_pre, b_c], [b_cv])
                    k.ACT(cvb[:], cvb[:], AF.Silu, [], [b_cv])
                    if g == 2:
                        k.CP(qkb[2][:], cvb[:], [b_cv], [b_qk[2]], eng='pool')
                    else:
                        for (t0, nb) in BLK:
                            k.ACT(sqb[:, 0:nb], cvb[:, t0:t0 + nb], AF.Square, [b_cv], [b_sqb])
                            k.MM(pss[:, 0:nb], onesb[:], sqb[:, 0:nb], [b_sqb, b_c], [b_pss])
                            k.TS(rs2[:, 0:nb], pss[:, 0:nb], 1e-6, None, ALU.add, None, [b_pss], [b_rs2])
                            k.ACT(rs2[:, 0:nb], rs2[:, 0:nb], AF.Sqrt, [], [b_rs2])
                            k.RCP(rs2[:, 0:nb], rs2[:, 0:nb], [], [b_rs2])
                            k.STT(qkb[g][:, t0:t0 + nb], cvb[:, t0:t0 + nb], (128 ** -0.5) if g == 0 else 1.0, rs2[:, 0:nb],
                                  ALU.mult, ALU.mult, [b_cv, b_rs2], [b_qk[g]])
                if stage == 1:
                    raise StopBuild()
                qT, kT, vT = qkb
                b_q, b_k, b_v = b_qk
                for d in range(2):
                    Tm, Um, Ng, Ms = (mk[:, 4 * d + j, :] for j in range(4))
                    ca = (0 + d) * 2 + hl
                    cb = (2 + d) * 2 + hl
                    col = d * 2 + hl
                    k.ACT(gt[:], abt[:, :, ca], AF.Exp, [b_c], [b_sc], bias=dtt[:, col:col + 1])
                    k.ACT(gt[:], gt[:], AF.Ln, [], [b_sc], bias=1.0)
                    k.TS(gt[:], gt[:], alt[:, col:col + 1], -1.0, ALU.mult, ALU.mult, [b_c], [b_sc])
                    k.ACT(bet[:], abt[:, :, cb], AF.Sigmoid, [b_c], [b_sc])
                    k.MM(psc[:, 0, 0:NTL], Tm, gt[:], [b_sc, b_c], [b_psc])
                    k.MM(psc[:, 1, 0:NTL], Um, gt[:], [b_sc, b_c], [b_psc])
                    k.MM(psc[:, 2, 0:NTL], ones[:], gt[:], [b_sc, b_c], [b_psc])
                    k.ACT(egc[:], psc[:, 0, 0:NTL], AF.Exp, [b_psc], [b_sc])
                    k.ACT(erest[:], psc[:, 1, 0:NTL], AF.Exp, [b_psc], [b_sc])
                    k.ACT(egl[:], psc[:, 2, 0:NTL], AF.Exp, [b_psc], [b_sc])
                    k.STT(nc1[:], bet[:], -1.0, egc[:], ALU.mult, ALU.mult, [], [b_sc])
                    k.MSET(S[:], 0.0, [], [b_S32]); k.MSET(Sb[:], 0.0, [], [b_S])
                    if stage == 2:
                        raise StopBuild()
                    order = list(range(NTL)) if d == 0 else [1, 0] + list(range(NTL - 1, 1, -1))
                    pairs = [(order[2 * n_], order[2 * n_ + 1]) for n_ in range(NTL // 2)]
                    for tis in pairs:
                        sls = [slice(t_ * 128, (t_ + 1) * 128) for t_ in tis]
                        for s_, ti in enumerate(tis):
                            k.TS(Tg2[:, s_, :], Tm, gt[:, ti:ti + 1], None, ALU.mult, None, [b_sc, b_c], [b_Tg], eng='pool')
                        for s_ in range(2):
                            k.MM(psA[:, s_, :], Tg2[:, s_, :], Um, [b_Tg, b_c], [b_psA], start=True, stop=False)
                            k.MM(psA[:, s_, :], idf[:], Ng, [b_c], [b_psA], start=False, stop=True)
                        for s_ in range(2):
                            k.MM(psA[:, 2 + s_, :], kT[:, sls[s_]], kT[:, sls[s_]], [b_k], [b_psA])
                        for s_ in range(2):
                            k.MM(psA2[:, s_, :], qT[:, sls[s_]], kT[:, sls[s_]], [b_q, b_k], [b_pss])
                        k.ACT(Dm2[:], psA[:, 0:2, :], AF.Exp, [b_psA], [b_Dm])
                        k.TT(DmS2[:], Dm2[:], Ms.unsqueeze(1).broadcast_to([128, 2, 128]), ALU.mult, [b_c], [b_Dm], eng='pool')
                        for s_, ti in enumerate(tis):
                            k.STT(Mm2[:, s_, :], psA[:, 2 + s_, :], bet[:, ti:ti + 1], DmS2[:, s_, :], ALU.mult, ALU.mult,
                                  [b_psA, b_Dm, b_sc], [b_MA])
                        k.TT(At2[:], psA2[:, 0:2, :], Dm2[:], ALU.mult, [b_pss, b_Dm], [b_MA])
                        for s_ in range(2):
                            k.TR(psT[:, 4 * s_ + 0, :], Mm2[:, s_, :], idb[:], [b_MA, b_c], [b_psT])
                            k.TR(psT[:, 4 * s_ + 1, :], At2[:, s_, :], idb[:], [b_MA, b_c], [b_psT])
                            k.TR(psT[:, 4 * s_ + 2, :], kT[:, sls[s_]], idb[:], [b_k, b_c], [b_psT])
                            k.TR(psT[:, 4 * s_ + 3, :], vT[:, sls[s_]], idb[:], [b_v, b_c], [b_psT])
                        k.CP(TRt2[:], psT[:], [b_psT], [b_TR], eng='act')
                        for s_, ti in enumerate(tis):
                            k.TS(bv2[:, s_, :], TRt2[:, 4 * s_ + 3, :], bet[:, ti:ti + 1], None, ALU.mult, None, [b_TR, b_sc], [b_bk], eng='pool')
                            k.TS(kdec2[:, s_, :], TRt2[:, 4 * s_ + 2, :], erest[:, ti:ti + 1], None, ALU.mult, None, [b_TR, b_sc], [b_bk], eng='pool')
                        k.TT(Cst2[:], Mm2[:].unsqueeze(2).broadcast_to([128, 2, 7, 128]),
                             bmk[:, 7 * d:7 * d + 7, :].unsqueeze(1).broadcast_to([128, 2, 7, 128]), ALU.mult, [b_MA, b_c], [b_Cst])
                        k.CP(DE32_2[:], II32_2[:], [b_c], [b_DE], eng='pool')
                        k.CP(DEb2[:], IIb_2[:], [b_c], [b_DEb], eng='pool')
                        for lv in range(7):
                            for s_ in range(2):
                                k.MM(psB[:, s_, :], Cst2[:, s_, lv, :], DEb2[:, s_, 0, :], [b_Cst, b_DEb], [b_psB])
                            k.CP(Gb2[:], psB[:, 0:2, :], [b_psB], [b_Gb], eng='act')
                            for s_ in range(2):
                                k.MM(psB2[:, s_, 0, :], DEb2[:, s_, 1, :], Gb2[:, s_, :], [b_DEb, b_Gb], [b_psc])
                                if lv < 6:
                                    k.MM(psB2[:, s_, 1, :], Gb2[:, s_, :], DEb2[:, s_, 1, :], [b_DEb, b_Gb], [b_psc])
                            if lv < 6:
                                k.TT(DE32_2[:], DE32_2[:], psB2[:], ALU.subtract, [b_psc], [b_DE])
                                k.CP(DEb2[:], DE32_2[:], [b_DE], [b_DEb], eng='act')
                            else:
                                k.TT(DEb2[:, :, 0, :], DE32_2[:, :, 0, :], psB2[:, :, 0, :], ALU.subtract, [b_psc, b_DE], [b_DEb])
                        cur = 0
                        b_XY = [b_DEb]
                        for s_, ti in enumerate(tis):
                            sl = sls[s_]
                            lat = ti >= 2
                            AinvT = DEb2[:, s_, 0, :]
                            k.MM(psS[:, 0, :], kT[:, sl], Sb[:], [b_k, b_S], [b_psS])
                            if lat:
                                k.MM(psS[:, 1, :], qT[:, sl], Sb[:], [b_q, b_S], [b_psS])
                                if True:
                                    k.TS(oq[:], psS[:, 1, :], egc[:, ti:ti + 1], None, ALU.mult, None, [b_psS, b_sc], [b_oq])
                                else:
                                    k.ACT(oq[:], psS[:, 1, :], AF.Copy, [b_psS, b_sc], [b_oq], scale=egc[:, ti:ti + 1])
                                if d == 1:
                                    k.TT(oq[:], oq[:], oacc[:, ti - 2, :], ALU.add, [b_pre], [b_oq], eng='pool')
                            k.STT(rr[:], psS[:, 0, :], nc1[:, ti:ti + 1], bv2[:, s_, :], ALU.mult, ALU.add, [b_psS, b_sc, b_bk], [b_rr])
                            k.MM(psS[:, 2, :], AinvT, rr[:], [b_XY[cur], b_rr], [b_psS])
                            k.CP(vn[:], psS[:, 2, :], [b_psS], [b_vn], eng='act')
                            k.MM(psU, kdec2[:, s_, :], vn[:], [b_bk, b_vn], [b_psU])
                            if lat:
                                k.MM(psS[:, 3, :], TRt2[:, 4 * s_ + 1, :], vn[:], [b_TR, b_vn], [b_psS])
                                if d == 0:
                                    if False:
                                        k.TT(ost[0][:], psS[:, 3, :], oq[:], ALU.add, [b_psS, b_oq], [b_ost[0]])
                                    else:
                                        k.TT(oacc[:, ti - 2, :], psS[:, 3, :], oq[:], ALU.add, [b_psS, b_oq], [b_pre])
                                else:
                                    i = ti % 2
                                    k.TT(ost[i][:], psS[:, 3, :], oq[:], ALU.add, [b_psS, b_oq], [b_ost[i]])
                                    k.TR(psO[:, 0, :], ost[i][:], idf[:], [b_ost[i], b_c], [b_psO])
                                    for s4 in range(4):
                                        e_ = nq[0] % 2; nq[0] += 1
                                        k.ACT(oex[e_][:], psO[:, 0, :], AF.Identity, [b_psO, b_c], [b_oex[e_]], scale=oht[:, s4:s4 + 1])
                                        k.DMA(GOi[s4 * 256 + hl * 128:s4 * 256 + (hl + 1) * 128, (ti - 2) * 128:(ti - 1) * 128], oex[e_][:],
                                              [b_oex[e_]], [T['b_GOi']], q='sp' if e_ == 0 else 'pool')
                            k.STT(Sb[:], S[:], egl[:, ti:ti + 1], psU, ALU.mult, ALU.add, [b_psU, b_sc, b_S32], [b_S])
                            k.STT(S[:], S[:], egl[:, ti:ti + 1], psU, ALU.mult, ALU.add, [b_psU, b_sc], [b_S32])
                            if stage == 3 or (stage == 4 and ti == 3) or (stage >= 100 and ti == stage - 100):
                                raise StopBuild()
            except StopBuild:
                pass
            k.wait_all('sp', [T['b_GOi'], b_S, b_S32, b_sc, b_qk[0], b_qk[1], b_qk[2]])
            k.emit()
    return nc


def phase_C(nc, k, T, st0):
    I = T['I']
    qnw = I("qnw", [128, 4]); wuqn = I("wuqn", [8, 512, 128]); wuqr = I("wuqr", [8, 512, 64]); wuqp = I("wuqp", [8, 512, 64])
    COSq = I("COSq", [64, NQ]); SINq = I("SINq", [64, NQ]); dnw = I("dnw", [128, 1])
    wa = I("wa", [16, 128, 8, 128]); wb = I("wb", [16, 128, 8, 128]); wo = I("wo", [16, 128, 16, 128])
    HW, XT, MOD, GKo, GVo, GOo, KR, X1T = T['HW'], T['XT'], T['MOD'], T['GKo'], T['GVo'], T['GOo'], T['KR'], T['X1T']
    bHW = T['b_HW']
    if True:
        yaT = st0.enter_context(nc.sbuf_tensor("c_yaT", [128, 8, NQ], BF16)); b_ya = Buf()
        with ExitStack() as st:
            sb = lambda n, s, dt=F32: st.enter_context(nc.sbuf_tensor("c_" + n, s, dt))
            ps = lambda n, s, dt=F32: st.enter_context(nc.psum_tensor("c_" + n, s, dt))
            b_c = Buf()
            ones = sb("ones", [128, 128]); onesb = sb("onesb", [128, 128], BF16)
            qnwt = sb("qnwt", [128, 4]); cosq = sb("cosq", [64, NQ]); sinq = sb("sinq", [64, NQ]); krs = sb("krs", [64, NT], BF16)
            k.MSET(ones[:], 1.0, [], [b_c]); k.MSET(onesb[:], 1.0, [], [b_c])
            for dst, src in [(qnwt, qnw), (cosq, COSq), (sinq, SINq)]:
                k.DMA(dst[:], src, [], [b_c])
            k.DMA(krs[:], KR, [T['b_KR']], [b_c])
            cqn = sb("cqn", [128, 4, NQ], BF16); b_cqn = Buf()
            xt = sb("cqx", [128, 4, 512]); b_xt = Buf()
            sq = sb("cqsq", [128, 512]); b_sq = Buf()
            rstd = sb("cqr", [128, 512]); b_rstd = Buf()
            pss = ps("pss", [128, 512]); b_pss = Buf()
            cv = HW[0:512, :].rearrange("(c p) t -> p c t", p=128)
            for qb in range(4):
                t0 = qb * 512
                k.DMA(xt[:], cv[:, :, 64 + t0:64 + t0 + 512], [bHW], [b_xt])
                for c in range(4):
                    k.ACT(sq[:], xt[:, c, :], AF.Square, [b_xt], [b_sq])
                    k.MM(pss[:], ones[:], sq[:], [b_sq, b_c], [b_pss], start=(c == 0), stop=(c == 3))
                k.TS(rstd[:], pss[:], 1.0 / 512, 1e-6, ALU.mult, ALU.add, [b_pss], [b_rstd])
                k.ACT(rstd[:], rstd[:], AF.Sqrt, [], [b_rstd])
                k.RCP(rstd[:], rstd[:], [], [b_rstd])
                for c in range(4):
                    k.STT(cqn[:, c, t0:t0 + 512], xt[:, c, :], qnwt[:, c:c + 1], rstd[:], ALU.mult, ALU.mult,
                          [b_xt, b_rstd, b_c], [b_cqn])
            Kh = [sb(f"Kh{i}", [128, NT], BF16) for i in range(2)]; Vh = [sb(f"Vh{i}", [128, NTL, 128], BF16) for i in range(2)]
            b_kv = [Buf(), Buf()]
            wst = sb("wst", [128, 4, 256]); wbf = [sb(f"wbf{i}", [128, 4, 256], BF16) for i in range(2)]; b_wst = Buf(); b_w = [Buf(), Buf()]
            qn = [sb(f"qn{i}", [128, 512], BF16) for i in range(2)]; qr = [sb(f"qr{i}", [64, 512], BF16) for i in range(2)]
            b_q = [Buf(), Buf()]
            rt = sb("rt", [64, 2, 512]); b_rt = Buf()
            PT = [sb(f"PT{i}", [128, 512], BF16) for i in range(3)]; b_PT = [Buf() for _ in range(3)]
            rz = sb("rz", [128, 512]); b_rz = Buf()
            zacc = [sb(f"zacc{i}", [128, 2, 512]) for i in range(2)]; b_zacc = [[Buf(), Buf()], [Buf(), Buf()]]
            pS = [ps(f"pS{i}", [128, 512]) for i in range(2)]; b_pS = [Buf(), Buf()]
            pO = ps("pO", [128, 512]); b_pO = Buf(); pZ = ps("pZ", [128, 512]); b_pZ = Buf()
            pQ = [ps(f"pQ{i}", [128, 512]) for i in range(3)]; b_pQ = [Buf() for _ in range(3)]
            it = 0
            for h in range(8):
                i = h % 2
                s_, hl_ = h // 2, h % 2
                k.DMA(Kh[i][:], GKo[s_ * 256 + hl_ * 128:s_ * 256 + (hl_ + 1) * 128, :], [T['b_GKo'][h]], [b_kv[i]], q='sp')
                vr0 = (s_ * 2 + hl_) * NTL * 128
                k.DMA(Vh[i][:], GVo[vr0:vr0 + NTL * 128, :].rearrange("(n p) d -> p n d", p=128), [T['b_GVo'][h]], [b_kv[i]], q='pool')
                for off, src, wd in [(0, wuqn, 128), (128, wuqr, 64), (192, wuqp, 64)]:
                    k.DMA(wst[:, :, off:off + wd], src[h].rearrange("(c p) n -> p c n", p=128), [], [b_wst])
                k.CP(wbf[i][:], wst[:], [b_wst], [b_w[i]], eng='pool')
                for qb in range(4):
                    t0 = qb * 512
                    j = (h * 4 + qb) % 2
                    for pi, (off, wd) in enumerate([(0, 128), (128, 64), (192, 64)]):
                        for c in range(4):
                            k.MM(pQ[pi][0:wd, :], wbf[i][:, c, off:off + wd], cqn[:, c, t0:t0 + 512], [b_w[i], b_cqn], [b_pQ[pi]],
                                 start=(c == 0), stop=(c == 3))
                    k.CP(qn[j][:], pQ[0][:], [b_pQ[0]], [b_q[j]], eng='act')
                    k.TT(rt[:, 0, :], pQ[1][0:64, :], cosq[:, t0:t0 + 512], ALU.mult, [b_pQ[1], b_c], [b_rt])
                    k.TT(rt[:, 1, :], pQ[2][0:64, :], sinq[:, t0:t0 + 512], ALU.mult, [b_pQ[2], b_c], [b_rt])
                    k.TT(qr[j][:], rt[:, 0, :], rt[:, 1, :], ALU.add, [b_rt], [b_q[j]], eng='pool')
                    for kt in range(NTL):
                        si = it % 2; pi = it % 3; it += 1
                        ks = slice(kt * 128, (kt + 1) * 128)
                        k.MM(pS[si][:], Kh[i][:, ks], qn[j][:], [b_kv[i], b_q[j]], [b_pS[si]], start=True, stop=False)
                        k.MM(pS[si][:], krs[:, ks], qr[j][:], [b_c, b_q[j]], [b_pS[si]], start=False, stop=True)
                        k.ACT(PT[pi][:], pS[si][:], AF.Exp, [b_pS[si]], [b_PT[pi]], scale=SCALE)
                        k.MM(pO[:], Vh[i][:, kt, :], PT[pi][:], [b_kv[i], b_PT[pi]], [b_pO], start=(kt == 0), stop=(kt == NTL - 1))
                        zp = kt % 2
                        if kt < 2:
                            k.CP(zacc[j][:, zp, :], PT[pi][:], [b_PT[pi]], [b_zacc[j][zp]], eng='dve')
                        else:
                            k.TT(zacc[j][:, zp, :], zacc[j][:, zp, :], PT[pi][:], ALU.add, [b_PT[pi]], [b_zacc[j][zp]], eng='dve')
                    k.MM(pZ[:], ones[:], zacc[j][:, 0, :], [b_c, b_zacc[j][0]], [b_pZ], start=True, stop=False)
                    k.MM(pZ[:], ones[:], zacc[j][:, 1, :], [b_c, b_zacc[j][1]], [b_pZ], start=False, stop=True)
                    k.RCP(rz[:], pZ[:], [b_pZ], [b_rz])
                    k.TT(yaT[:, h, t0:t0 + 512], pO[:], rz[:], ALU.mult, [b_pO, b_rz], [b_ya])
            k.emit()
        with ExitStack() as st:
            sb = lambda n, s, dt=F32: st.enter_context(nc.sbuf_tensor("c_" + n, s, dt))
            ps = lambda n, s, dt=F32: st.enter_context(nc.psum_tensor("c_" + n, s, dt))
            b_c = Buf()
            ones = sb("ones3", [128, 128]); dnwt = sb("dnwt", [128, 1]); g1t = sb("g1t", [128, 16]); oht = sb("oht3", [128, 4])
            k.MSET(ones[:], 1.0, [], [b_c]); k.DMA(dnwt[:], dnw, [], [b_c]); k.DMA(g1t[:], MOD[:, 0, 32:48], [T["b_MOD"]], [b_c])
            k.DMA(oht[:], T["oh4"], [], [b_c])
            ocd = [sb(f"ocd{i}", [128, 512], BF16) for i in range(2)]; b_ocd = [Buf(), Buf()]; no = [0]
            wso = [sb(f"wso{i}", [128, 16, 128]) for i in range(2)]; wbo = [sb(f"wbo{i}", [128, 16, 128], BF16) for i in range(2)]
            b_wso = [Buf(), Buf()]; b_wbo = [Buf(), Buf()]
            ybT = sb("ybT", [128, 8, 512], BF16); b_yb = Buf()
            mg = sb("mg", [128, 16, 512], BF16); b_mg = Buf()
            os_ = [sb(f"os{i}", [128, 512]) for i in range(2)]; zz = [sb(f"zz{i}", [128, 512]) for i in range(2)]; b_oz = [Buf(), Buf()]
            sq = sb("sq3", [128, 512]); b_sq = Buf(); rs = sb("rs3", [128, 512]); b_rs = Buf()
            wst = [sb(f"wst3{i}", [128, 8, 256]) for i in range(2)]; wbf = [sb(f"wbf3{i}", [128, 8, 256], BF16) for i in range(2)]
            b_wst = [Buf(), Buf()]; b_wbf = [Buf(), Buf()]
            gt = [sb(f"gt3{i}", [128, 2, 512]) for i in range(2)]; b_gt = [Buf(), Buf()]
            t1 = sb("t13", [128, 512]); t2 = sb("t23", [128, 512]); b_t = Buf()
            xs = [sb(f"xs{i}", [128, 512]) for i in range(2)]; b_xs = [Buf(), Buf()]
            ys = [sb(f"ys{i}", [128, 512]) for i in range(2)]; b_ys = [Buf(), Buf()]
            pss = ps("pss3", [128, 512]); b_pss = Buf()
            pA = ps("pA", [128, 512]); pB = ps("pB", [128, 512]); b_pA = Buf(); b_pB = Buf()
            pY = [ps(f"pY{i}", [128, 512]) for i in range(2)]; b_pY = [Buf(), Buf()]
            zv = HW[3904:4928, :].rearrange("(c p) t -> p c t", p=128)
            gav = HW[4960:7008, :].rearrange("(c p) t -> p c t", p=128); gbv = HW[7008:9056, :].rearrange("(c p) t -> p c t", p=128)
            XTv = XT.rearrange("(c p) t -> p c t", p=128); X1v = X1T.rearrange("(c p) t -> p c t", p=128)
            for qb in range(4):
                t0 = qb * 512
                for hh in range(8):
                    i = hh % 2
                    for jc in range(4):
                        ci = no[0] % 2; no[0] += 1
                        orow = (hh // 2) * 256 + (hh % 2) * 128
                        k.DMA(ocd[ci][:], GOo[orow:orow + 128, jc * 2048 + t0:jc * 2048 + t0 + 512], [T['b_GOo'][hh]], [b_ocd[ci]], q='sp')
                        if jc == 0:
                            k.TS(os_[i][:], ocd[ci][:], oht[:, 0:1], None, ALU.mult, None, [b_ocd[ci], b_c], [b_oz[i]])
                        else:
                            k.STT(os_[i][:], ocd[ci][:], oht[:, jc:jc + 1], os_[i][:], ALU.mult, ALU.add, [b_ocd[ci], b_c], [b_oz[i]])
                    k.DMA(zz[i][:], zv[:, hh, 64 + t0:64 + t0 + 512], [bHW], [b_oz[i]], q='pool')
                    k.ACT(sq[:], os_[i][:], AF.Square, [b_oz[i]], [b_sq])
                    k.MM(pss[:], ones[:], sq[:], [b_sq, b_c], [b_pss])
                    k.TS(rs[:], pss[:], 1.0 / 128, 1e-6, ALU.mult, ALU.add, [b_pss], [b_rs])
                    k.ACT(rs[:], rs[:], AF.Sqrt, [], [b_rs])
                    k.RCP(rs[:], rs[:], [], [b_rs])
                    k.ACT(zz[i][:], zz[i][:], AF.Silu, [], [b_oz[i]])
                    k.TT(rs[:], rs[:], os_[i][:], ALU.mult, [b_oz[i]], [b_rs])
                    k.STT(ybT[:, hh, :], rs[:], dnwt[:, 0:1], zz[i][:], ALU.mult, ALU.mult, [b_rs, b_oz[i], b_c], [b_yb])
                for m in range(16):
                    i = m % 2
                    ms = slice(m * 128, (m + 1) * 128)
                    k.DMA(wbf[i][:, :, 0:128], wa[m], [], [b_wbf[i]], q='pool')
                    k.DMA(wbf[i][:, :, 128:256], wb[m], [], [b_wbf[i]], q='pool')
                    k.DMA(gt[i][:, 0, :], gav[:, m, 64 + t0:64 + t0 + 512], [bHW], [b_gt[i]], q='sp')
                    k.DMA(gt[i][:, 1, :], gbv[:, m, 64 + t0:64 + t0 + 512], [bHW], [b_gt[i]], q='pool')
                    k.ACT(gt[i][:], gt[i][:], AF.Sigmoid, [], [b_gt[i]])
                    for c in range(8):
                        k.MM(pA[:], wbf[i][:, c, 0:128], yaT[:, c, t0:t0 + 512], [b_wbf[i], b_ya], [b_pA], start=(c == 0), stop=(c == 7))
                    for c in range(8):
                        k.MM(pB[:], wbf[i][:, c, 128:256], ybT[:, c, :], [b_wbf[i], b_yb], [b_pB], start=(c == 0), stop=(c == 7))
                    k.TT(t1[:], pA[:], gt[i][:, 0, :], ALU.mult, [b_pA, b_gt[i]], [b_t])
                    k.TT(t2[:], pB[:], gt[i][:, 1, :], ALU.mult, [b_pB, b_gt[i]], [b_t])
                    k.TT(mg[:, m, :], t1[:], t2[:], ALU.add, [b_t], [b_mg], eng='pool')
                for dc in range(16):
                    i = dc % 2
                    k.DMA(wbo[i][:], wo[dc], [], [b_wbo[i]], q='pool')
                    k.DMA(xs[i][:], XTv[:, dc, 64 + t0:64 + t0 + 512], [T['b_XT']], [b_xs[i]])
                    for m in range(16):
                        k.MM(pY[i][:], wbo[i][:, m, :], mg[:, m, :], [b_mg, b_wbo[i]], [b_pY[i]], start=(m == 0), stop=(m == 15))
                    k.STT(ys[i][:], pY[i][:], g1t[:, dc:dc + 1], xs[i][:], ALU.mult, ALU.add, [b_pY[i], b_c, b_xs[i]], [b_ys[i]])
                    k.DMA(X1v[:, dc, t0:t0 + 512], ys[i][:], [b_ys[i]], [T['b_X1T']], q='pool')
            k.wait_all('sp', [T['b_X1T']])
            k.emit()


def phase_D(nc, k, T, st, nblk=NBLK):
    I = T['I']
    n2w = I("n2w", [128, 16]); fnw = I("fnw", [128, 16]); wq = I("wq", [16, 128, 16, 128]); skT = I("skT", [128, 2, 128])
    uT = I("uT", [128, 128, 16, 128]); v = I("v", [16384, 2048]); iota = I("iota", [128, 128]); ident = T['ident']
    x1T, MOD, out = T['X1T'], T['MOD'], T['out']
    UB, VB = T['UB'], T['VB']
    b_UB = [Buf() for _ in range(128)]; b_VB = [[Buf() for _ in range(128)] for _ in range(2)]
    if True:
        sb = lambda n, s, dt=F32: st.enter_context(nc.sbuf_tensor("d_" + n, s, dt))
        b_out = Buf(); b_c = Buf()
        P = [st.enter_context(nc.psum_tensor(f"d_P{i}", [128, 512], F32)) for i in range(8)]
        bP = [Buf() for _ in range(8)]
        ones = sb("ones", [128, 128]); n2t = sb("n2t", [128, 16]); m2t = sb("m2t", [128, 3, 16]); fnt = sb("fnt", [128, 16])
        skt = sb("skt", [128, 2, 128]); iot = sb("iot", [128, 128]); iob = sb("iob", [128, 128], BF16); idt = sb("idt", [128, 128])
        scl2 = sb("scl2", [128, 16])
        k.MSET(ones[:], 1.0, [], [b_c])
        for dst, src in [(n2t, n2w), (fnt, fnw), (skt, skT), (iot, iota), (idt, ident)]:
            k.DMA(dst[:], src, [], [b_c])
        for q_ in range(3):
            k.DMA(m2t[:, q_, :], MOD[:, 0, 48 + 16 * q_:64 + 16 * q_], [T['b_MOD']], [b_c])
        k.CP(iob[:], iot[:], [b_c], [b_c])
        k.STT(scl2[:], m2t[:, 1, :], 1.0, n2t[:], ALU.add, ALU.mult, [b_c], [b_c])
        xb = sb("xb", [128, 16, TB]); b_xb = Buf()
        sq = sb("sq", [128, TB]); b_sq = Buf(); rstd = sb("rstd", [128, TB]); b_rstd = Buf()
        h2 = sb("h2", [128, 16, TB], BF16); b_h2 = Buf()
        wbf = [sb(f"wbf{i}", [128, 16, 128], BF16) for i in range(4)]; b_wbf = [Buf() for _ in range(4)]
        qT = sb("qT", [128, 16, TB]); b_qT = Buf()
        sc = [sb(f"sc{i}", [128, 16, 128]) for i in range(2)]; scw = [sb(f"scw{i}", [128, 128]) for i in range(2)]; b_sc = [Buf(), Buf()]; b_scw = [Buf(), Buf()]
        tv = [sb(f"tv{i}", [128, 16, 16]) for i in range(2)]; ti = [sb(f"ti{i}", [128, 16, 16], U32) for i in range(2)]
        tif = [sb(f"tif{i}", [128, 16, 16]) for i in range(2)]; b_tv = [Buf(), Buf()]
        cand = [sb(f"cand{i}", [128, 8, 256]) for i in range(2)]; cw_ = [sb(f"candw{i}", [128, 256]) for i in range(2)]; b_cand = [Buf(), Buf()]; b_cw = [Buf(), Buf()]
        bs = [sb(f"bs{i}", [128, 8, 16]) for i in range(2)]; bp = [sb(f"bp{i}", [128, 8, 16], U32) for i in range(2)]; ba = sb("ba", [128, 8, 16], U32); bb = sb("bb", [128, 8, 16], U32)
        af = sb("af", [128, 8, 16]); bf = sb("bf", [128, 8, 16]); b_bs = [Buf(), Buf()]; b_bab = Buf()
        eq = sb("eq", [128, 8, 16, 16]); b_eq = Buf()
        ijw = sb("ijw", [128, 3, 8, 16]); b_ijw = Buf()
        ssum = sb("ssum", [128, 8]); b_ss = Buf()
        IJW = sb("IJW", [128, 3, TB]); b_IJW = Buf()
        NB = 16
        oh = [sb(f"oh{i}", [128, 2, NB, 128], BF16) for i in range(2)]; b_oh = [Buf(), Buf()]
        CT = sb("CT", [128, 128, TB], BF16); b_CT = Buf()
        gl = [sb(f"gl{i}", [128, TB]) for i in range(2)]; b_gl = [Buf(), Buf()]
        scb = [sc[i_][:].rearrange("p a b -> p (a b)").bitcast(BF16) for i_ in range(2)]
        vbf = [scb[0][:, 0:2048], scb[0][:, 2048:4096], scb[1][:, 0:2048], scb[1][:, 2048:4096]]; b_vbf = [Buf() for _ in range(4)]
        dmy = sb("dmy", [128, 8])
        b_orow = [Buf(), Buf()]
        xv = x1T.rearrange("(c p) t -> p c t", p=128)
        for blk in range(nblk):
            t0 = blk * TB
            k.DMA(xb[:], xv[:, :, t0:t0 + TB], [T['b_X1T']], [b_xb])
            for c in range(16):
                k.ACT(sq[:], xb[:, c, :], AF.Square, [b_xb], [b_sq])
                k.MM(P[0][:, 0:TB], ones[:], sq[:], [b_sq, b_c], [bP[0]], start=(c == 0), stop=(c == 15))
            k.TS(rstd[:], P[0][:, 0:TB], 1.0 / 2048, 1e-6, ALU.mult, ALU.add, [bP[0]], [b_rstd])
            k.ACT(rstd[:], rstd[:], AF.Sqrt, [], [b_rstd])
            k.RCP(rstd[:], rstd[:], [], [b_rstd])
            for c in range(16):
                k.TT(sq[:], xb[:, c, :], rstd[:], ALU.mult, [b_xb, b_rstd], [b_sq])
                k.ACT(h2[:, c, :], sq[:], AF.Identity, [b_sq, b_c], [b_h2], scale=scl2[:, c:c + 1], bias=m2t[:, 0, c:c + 1])
            for cq in range(16):
                i = cq % 2
                k.DMA(wbf[i][:], wq[cq], [], [b_wbf[i]], q='pool')
                pb = 1 + (cq % 2)
                for c in range(16):
                    k.MM(P[pb][:, 0:TB], wbf[i][:, c, :], h2[:, c, :], [b_wbf[i], b_h2], [bP[pb]], start=(c == 0), stop=(c == 15))
                k.CP(qT[:, cq, :], P[pb][:, 0:TB], [bP[pb]], [b_qT], eng='act')
            NTT = TB // 128
            for tt in range(NTT):
                ts_ = slice(tt * 128, (tt + 1) * 128)
                for cq in range(16):
                    pb = 3 + (cq // 4) % 2
                    k.MM(P[pb][:, (cq % 4) * 128:(cq % 4 + 1) * 128], qT[:, cq, ts_], skt[:, cq % 2, :], [b_qT, b_c], [bP[pb]])
                    if cq % 4 == 3:
                        k.CP(sc[tt][:, cq - 3:cq + 1, :], P[pb][:].rearrange("p (a b) -> p a b", b=128), [bP[pb]], [b_sc[tt]], eng='act')
            for cq in range(16):
                for tt in range(NTT):
                    k.dve(lambda e, cq=cq, tt=tt: e.max(out=tv[tt][:, cq, 0:8], in_=sc[tt][:, cq, :]), [b_sc[tt]], [b_tv[tt]])
                    k.dve(lambda e, cq=cq, tt=tt: e.max_index(out=ti[tt][:, cq, 0:8], in_max=tv[tt][:, cq, 0:8], in_values=sc[tt][:, cq, :]),
                          [b_sc[tt]], [b_tv[tt]])
                    k.dve(lambda e, cq=cq, tt=tt: e.match_replace(out=scw[tt][:], in_to_replace=tv[tt][:, cq, 0:8], in_values=sc[tt][:, cq, :],
                                                                  imm_value=NEG), [b_sc[tt], b_tv[tt]], [b_scw[tt]])
                    k.dve(lambda e, cq=cq, tt=tt: e.max(out=tv[tt][:, cq, 8:16], in_=scw[tt][:]), [b_scw[tt]], [b_tv[tt]])
                    k.dve(lambda e, cq=cq, tt=tt: e.max_index(out=ti[tt][:, cq, 8:16], in_max=tv[tt][:, cq, 8:16], in_values=scw[tt][:]),
                          [b_scw[tt]], [b_tv[tt]])
            for tt in range(NTT):
                k.CP(tif[tt][:], ti[tt][:], [b_tv[tt]], [b_tv[tt]])
                for h in range(8):
                    k.TT(cand[tt][:, h, :].rearrange("p (a b) -> p a b", b=16), tv[tt][:, 2 * h, :].unsqueeze(2).broadcast_to([128, 16, 16]),
                         tv[tt][:, 2 * h + 1, :].unsqueeze(1).broadcast_to([128, 16, 16]), ALU.add, [b_tv[tt]], [b_cand[tt]], eng='pool')
            for h in range(8):
                for tt in range(NTT):
                    k.dve(lambda e, h=h, tt=tt: e.max(out=bs[tt][:, h, 0:8], in_=cand[tt][:, h, :]), [b_cand[tt]], [b_bs[tt]])
                    k.dve(lambda e, h=h, tt=tt: e.max_index(out=bp[tt][:, h, 0:8], in_max=bs[tt][:, h, 0:8], in_values=cand[tt][:, h, :]),
                          [b_cand[tt]], [b_bs[tt]])
                    k.dve(lambda e, h=h, tt=tt: e.match_replace(out=cw_[tt][:], in_to_replace=bs[tt][:, h, 0:8], in_values=cand[tt][:, h, :],
                                                                imm_value=NEG), [b_cand[tt], b_bs[tt]], [b_cw[tt]])
                    k.dve(lambda e, h=h, tt=tt: e.max(out=bs[tt][:, h, 8:16], in_=cw_[tt][:]), [b_cw[tt]], [b_bs[tt]])
                    k.dve(lambda e, h=h, tt=tt: e.max_index(out=bp[tt][:, h, 8:16], in_max=bs[tt][:, h, 8:16], in_values=cw_[tt][:]),
                          [b_cw[tt]], [b_bs[tt]])
            for tt in range(NTT):
                ts_ = slice(tt * 128, (tt + 1) * 128)
                tif4 = tif[tt][:].rearrange("p (h two) a -> p h two a", two=2)
                k.TS(ba[:], bp[tt][:], 4, None, ALU.logical_shift_right, None, [b_bs[tt]], [b_bab])
                k.TS(bb[:], bp[tt][:], 15, None, ALU.bitwise_and, None, [b_bs[tt]], [b_bab])
                k.CP(af[:], ba[:], [], [b_bab]); k.CP(bf[:], bb[:], [], [b_bab])
                for which, sel in ((0, af), (1, bf)):
                    k.TT(eq[:], sel[:].unsqueeze(3).broadcast_to([128, 8, 16, 16]),
                         iot[:, 0:16].unsqueeze(1).unsqueeze(1).broadcast_to([128, 8, 16, 16]), ALU.is_equal, [b_bab, b_c], [b_eq])
                    k.TT(eq[:], eq[:], tif4[:, :, which, :].unsqueeze(2).broadcast_to([128, 8, 16, 16]), ALU.mult, [b_tv[tt]], [b_eq])
                    k.dve(lambda e, which=which: e.tensor_reduce(out=ijw[:, which, :, :], in_=eq[:], op=ALU.add, axis=mybir.AxisListType.X),
                          [b_eq], [b_ijw])
                k.TT(ijw[:, 2, :, :], bs[tt][:], bs[tt][:, :, 0:1].broadcast_to([128, 8, 16]), ALU.subtract, [b_bs[tt]], [b_ijw])
                k.ACT(ijw[:, 2, :, :], ijw[:, 2, :, :], AF.Exp, [], [b_ijw])
                k.dve(lambda e: e.tensor_reduce(out=ssum[:], in_=ijw[:, 2, :, :], op=ALU.add, axis=mybir.AxisListType.X), [b_ijw], [b_ss])
                k.RCP(ssum[:], ssum[:], [], [b_ss])
                k.TT(ijw[:, 2, :, :], ijw[:, 2, :, :], ssum[:].unsqueeze(2).broadcast_to([128, 8, 16]), ALU.mult, [b_ss], [b_ijw])
                for q_ in range(3):
                    k.TR(P[5][:, q_ * 128:(q_ + 1) * 128], ijw[:, q_, :, :].rearrange("p h k -> p (h k)"), idt[:], [b_ijw, b_c], [bP[5]])
                k.CP(IJW[:, :, ts_], P[5][:, 0:384].rearrange("p (a b) -> p a b", b=128), [bP[5]], [b_IJW], eng='act')
            for g in range(TB // NB):
                i = g % 2
                g0 = g * NB
                k.TT(oh[i][:, 0, :, :], iob[:].unsqueeze(1).broadcast_to([128, NB, 128]),
                     IJW[:, 1, g0:g0 + NB].unsqueeze(2).broadcast_to([128, NB, 128]), ALU.is_equal, [b_IJW, b_c], [b_oh[i]])
                k.TT(oh[i][:, 1, :, :], iob[:].unsqueeze(1).broadcast_to([128, NB, 128]),
                     IJW[:, 0, g0:g0 + NB].unsqueeze(2).broadcast_to([128, NB, 128]), ALU.is_equal, [b_IJW, b_c], [b_oh[i]])
                k.TT(oh[i][:, 1, :, :], oh[i][:, 1, :, :], IJW[:, 2, g0:g0 + NB].unsqueeze(2).broadcast_to([128, NB, 128]), ALU.mult,
                     [b_IJW], [b_oh[i]])
                for q4 in range(NB // 4):
                    pb = 6 + (q4 % 2)
                    for s_ in range(4):
                        tk = q4 * 4 + s_
                        k.MM(P[pb][:, s_ * 128:(s_ + 1) * 128], oh[i][:, 0, tk, :], oh[i][:, 1, tk, :], [b_oh[i]], [bP[pb]])
                    tg = g0 + q4 * 4
                    k.CP(CT[:, :, tg:tg + 4].rearrange("p i t -> p t i"), P[pb][:].rearrange("p (t i) -> p t i", i=128), [bP[pb]], [b_CT],
                         eng='act' if q4 % 2 == 0 else 'dve')
            for ie in range(128):
                i = ie % 4
                j2 = ie % 2
                if blk == 0:
                    k.DMA(wbf[i][:], uT[ie], [], [b_wbf[i]], q='pool')
                    if nblk > 1:
                        k.DMA(UB[ie], wbf[i][:], [b_wbf[i]], [b_UB[ie]], q='sp')
                else:
                    k.DMA(wbf[i][:], UB[ie], [b_UB[ie]], [b_wbf[i]], q=('sp', 'act', 'pool')[ie % 3])
                for c in range(16):
                    k.MM(P[j2][:, 0:TB], wbf[i][:, c, :], h2[:, c, :], [b_wbf[i], b_h2], [bP[j2]], start=(c == 0), stop=(c == 15))
                k.ACT(gl[j2][:], P[j2][:, 0:TB], AF.Gelu, [bP[j2]], [b_gl[j2]])
                k.TT(CT[:, ie, :], gl[j2][:], CT[:, ie, :], ALU.mult, [b_gl[j2]], [b_CT], eng='dve' if j2 == 0 else 'pool')
            k.pool(lambda e: e.memset(dmy[:], 0.0), [b_sc[0], b_sc[1], b_scw[0], b_scw[1], b_tv[0], b_tv[1]], b_vbf)
            for ie in range(128):
                i = ie % 4
                vsl = (slice(ie * 128, (ie + 1) * 128), slice(0, 2048))
                if blk == 0:
                    k.DMA(vbf[i], v[vsl], [], [b_vbf[i]], q='pool')
                    if nblk > 1:
                        k.DMA(VB[vsl], vbf[i], [b_vbf[i]], [b_VB[0][ie]], q='sp')
                else:
                    k.DMA(vbf[i], VB[vsl], [b_VB[0][ie]], [b_vbf[i]], q=('sp', 'act', 'pool')[ie % 3])
                for dc in range(16):
                    k.mm(lambda e, i=i, dc=dc, ie=ie: e.matmul(P[dc // 2][:, (dc % 2) * TB:(dc % 2 + 1) * TB], lhsT=vbf[i][:, dc * 128:(dc + 1) * 128],
                                                          rhs=CT[:, ie, :], start=(ie == 0 and dc % 2 == 0), stop=(ie == 127),
                                                          skip_group_check=True), [b_vbf[i], b_CT], [bP[dc // 2]])
            for c in range(16):
                k.STT(xb[:, c, :], P[c // 2][:, (c % 2) * TB:(c % 2 + 1) * TB], m2t[:, 2, c:c + 1], xb[:, c, :], ALU.mult, ALU.add,
                      [bP[c // 2], b_c], [b_xb])
            k.pool(lambda e: e.memset(dmy[:], 0.0), b_vbf, [b_sc[0], b_sc[1]])
            for c in range(16):
                k.ACT(sq[:], xb[:, c, :], AF.Square, [b_xb], [b_sq])
                k.MM(P[0][:, 0:TB], ones[:], sq[:], [b_sq, b_c], [bP[0]], start=(c == 0), stop=(c == 15))
            k.TS(rstd[:], P[0][:, 0:TB], 1.0 / 2048, 1e-6, ALU.mult, ALU.add, [bP[0]], [b_rstd])
            k.ACT(rstd[:], rstd[:], AF.Sqrt, [], [b_rstd])
            k.RCP(rstd[:], rstd[:], [], [b_rstd])
            for c in range(16):
                k.STT(xb[:, c, :], xb[:, c, :], fnt[:, c:c + 1], rstd[:], ALU.mult, ALU.mult, [b_rstd, b_c], [b_xb])
            for tt in range(TB // 128):
                i = tt % 2
                for c in range(16):
                    pb = (c // 4) % 2
                    k.TR(P[pb][:, (c % 4) * 128:(c % 4 + 1) * 128], xb[:, c, tt * 128:(tt + 1) * 128], idt[:], [b_xb, b_c], [bP[pb]])
                    if c % 4 == 3:
                        k.CP(qT[:, 8 * i:8 * i + 8, :].rearrange("p a b -> p (a b)")[:, (c - 3) * 128:(c + 1) * 128], P[pb][:], [bP[pb]], [b_orow[i], b_qT],
                             eng='act' if pb == 0 else 'dve')
                k.DMA(out[t0 + tt * 128:t0 + (tt + 1) * 128, :], qT[:, 8 * i:8 * i + 8, :].rearrange("p a b -> p (a b)"), [b_orow[i], b_qT], [b_out],
                      q='sp' if i == 0 else 'pool')
        k.wait_all('sp', [b_out])
        k.emit()


def build_F(upto=4):
    nc = bass.Bass('TRN2', target_bir_lowering=False)
    ins = {}

    def I(n, s, dt=F32):
        if n not in ins:
            ins[n] = nc.dram_tensor(n, s, dt, kind="ExternalInput").ap()
        return ins[n]
    T = {'I': I, 'ins': ins}
    T['ident'] = I("ident", [128, 128]); T['oh4'] = I("oh4", [128, 4])
    T['out'] = nc.dram_tensor("out", [NQ, D], F32, kind="ExternalOutput").ap()

    def scr(name, shape, dt=F32):
        T[name] = nc.dram_tensor("scr_" + name, shape, dt).ap()
        T['b_' + name] = Buf()
    scr('XT', [D, NTOK]); scr('HW', [MCH * 128, NTOK]); scr('MOD', [128, 2, 96])
    scr('G1i', [4 * GR, NTOK], BF16); scr('G1o', [4 * GR, NTOK], BF16); scr('G1fi', [128, NTOK]); scr('G1fo', [128, NTOK])
    scr('GKi', [1024, NT], BF16); scr('GKo', [1024, NT], BF16)
    scr('GVi', [4 * 2 * NTL * 128, 128], BF16); scr('GVo', [4 * 2 * NTL * 128, 128], BF16)
    scr('GOi', [1024, 8192], BF16); scr('GOo', [1024, 8192], BF16)
    scr('KR', [64, NT], BF16); scr('X1T', [D, NQ])
    scr('UB', [128, 128, 16, 128], BF16); scr('VB', [16384, 2048], BF16)
    with ExitStack() as st0:
        k = Ctx(nc, st0)
        phase_A(nc, k, T)
        T['b_G1o'] = [Buf() for _ in range(27)]
        first = sorted({(s_ * GR + r_) // 512 for s_ in range(4) for r_ in (0, 319)})
        for c_ in first + [c_ for c_ in range(27) if c_ not in first]:
            k.coll(lambda e, c_=c_: e.collective_compute("AllReduce", ALU.add, replica_groups=RG, ins=[T['G1i'][c_ * 512:(c_ + 1) * 512, :].opt()],
                                                         outs=[T['G1o'][c_ * 512:(c_ + 1) * 512, :].opt()]), [T['b_G1i']], [T['b_G1o'][c_]])
        allreduce_chunks(k, T['G1fi'], T['G1fo'], 128, 128, [T['b_G1fi']], [T['b_G1fo']])
        if upto >= 2:
            phase_B(nc, k, T)
            for nm in ('GKo', 'GVo', 'GOo'):
                T['b_' + nm] = [Buf() for _ in range(8)]
            for h in range(8):
                for nm, ch in (('GK', 128), ('GV', NT)):
                    k.coll(lambda e, nm=nm, ch=ch, h=h: e.collective_compute(
                        "AllReduce", ALU.add, replica_groups=RG, ins=[T[nm + 'i'][h * ch:(h + 1) * ch, :].opt()],
                        outs=[T[nm + 'o'][h * ch:(h + 1) * ch, :].opt()]), [T['b_' + nm + 'i']], [T['b_' + nm + 'o'][h]])
            for h in range(8):
                k.coll(lambda e, h=h: e.collective_compute(
                    "AllReduce", ALU.add, replica_groups=RG, ins=[T['GOi'][h * 128:(h + 1) * 128, :].opt()],
                    outs=[T['GOo'][h * 128:(h + 1) * 128, :].opt()]), [T['b_GOi']], [T['b_GOo'][h]])
        if upto >= 3:
            with ExitStack() as stc:
                phase_C(nc, k, T, stc)
        if upto >= 4:
            with ExitStack() as std:
                phase_D(nc, k, T, std)
        else:
            k.wait_all('pool', T['b_G1o'] + [T['b_G1fo']] + T['b_GKo'] + T['b_GVo'] + T['b_GOo'])
            k.emit()
    return nc, T


def host_F(inp):
    cos, sin = rope_tables()
    COSf = np.concatenate([np.ones((64, 256), np.float32), cos], 1)
    SINf = np.concatenate([np.zeros((64, 256), np.float32), sin], 1)
    masks, ident, bmask = gdn_consts()
    fm = lambda vec: np.ascontiguousarray(vec.reshape(16, 128).T)

    def chunked(w):
        K, N = w.shape
        Np = -(-N // 128) * 128
        if Np != N:
            w = np.concatenate([w, np.zeros((K, Np - N), w.dtype)], 1)
        return np.ascontiguousarray(w.reshape(K // 128, 128, Np // 128, 128).transpose(2, 1, 0, 3))
    wuq = inp['w_uq'][0]
    wuqn = np.ascontiguousarray(np.stack([wuq[:, h * 192:h * 192 + 128] for h in range(8)]))
    wuqr = np.ascontiguousarray(np.stack([wuq[:, h * 192 + 128:h * 192 + 192] for h in range(8)]))
    wuqp = np.ascontiguousarray(wuqr[:, :, PERM])
    sk = inp['peer_sub_keys'][0]
    common = {
        "w_mod": chunked(inp['w_mod'][0]), "b_mod": np.ascontiguousarray(inp['b_mod'][0].reshape(96, 128).T),
        "n1w": fm(inp['norm1_w'][0]), "w_in": chunked(inp['w_in'][0]), "ident": ident,
        "COS": COSf, "SIN": SINf, "kvw": np.ascontiguousarray(inp['mla_kv_norm_w'][0].reshape(2, 128).T),
        "masks": masks, "bmask": bmask, "qnw": np.ascontiguousarray(inp['mla_q_norm_w'][0].reshape(4, 128).T),
        "wuqn": wuqn, "wuqr": wuqr, "wuqp": wuqp, "dnw": np.ascontiguousarray(inp['dn_norm_w'][0].reshape(128, 1)),
        "wa": chunked(inp['w_branch_a'][0]), "wb": chunked(inp['w_branch_b'][0]),
        "wo": chunked(inp['w_out'][0]), "n2w": fm(inp['norm2_w'][0]), "fnw": fm(inp['final_norm_w']),
        "wq": chunked(inp['peer_w_q'][0]), "skT": np.ascontiguousarray(sk.transpose(2, 0, 1)),
        "uT": chunked(inp['peer_u'][0].T), "v": np.ascontiguousarray(inp['peer_v'][0]),
        "iota": np.ascontiguousarray(np.broadcast_to(np.arange(128, dtype=np.float32), (128, 128))),
    }
    maps = []
    for core in range(8):
        b, j = core // 4, core % 4
        tok = slice(2048 * j, 2048 * j + 2048)
        cw = inp['dn_conv_w'][0]
        convw = np.stack([cw[:, g * 1024 + (2 * j + hl) * 128: g * 1024 + (2 * j + hl + 1) * 128].T for hl in range(2) for g in range(3)], 1)
        al = np.array([inp['dn_a_log'][0, d, 2 * j + hl] for d in range(2) for hl in range(2)], np.float32)
        dtb = np.array([inp['dn_dt_bias'][0, d, 2 * j + hl] for d in range(2) for hl in range(2)], np.float32)
        m = dict(common)
        m.update({
            "x_own": np.ascontiguousarray(np.concatenate([inp['ctx'][b, 64 * j:64 * j + 64], inp['x'][b, tok]], 0)),
            "cc": np.ascontiguousarray(np.stack([inp['c'][b], inp['c_ctx']], -1).reshape(16, 128, 2).transpose(1, 0, 2)),
            "oh4": np.ascontiguousarray(np.broadcast_to(np.eye(4, dtype=np.float32)[j], (128, 4))),
            "wukv": np.ascontiguousarray(inp['w_ukv'][0][:, 2 * j * 256:(2 * j + 2) * 256]),
            "convw": np.ascontiguousarray(convw), "alog": np.ascontiguousarray(np.broadcast_to(al, (128, 4))),
            "dtb": np.ascontiguousarray(np.broadcast_to(dtb, (128, 4))),
            "COSq": np.ascontiguousarray(cos[:, tok]), "SINq": np.ascontiguousarray(sin[:, tok]),
        })
        maps.append(m)
    return maps


_CORES = list(range(8))


def kernel(**inputs):
    inp = {k_: np.asarray(v_) for k_, v_ in inputs.items()}
    nc, T = build_F()
    maps = [{k_: v_ for k_, v_ in m.items() if k_ in T['ins']} for m in host_F(inp)]
    res = run_bass_kernel_spmd(nc, maps, core_ids=_CORES).results
    out = np.empty((2, 8192, 2048), np.float32)
    for core in range(8):
        b, j = core // 4, core % 4
        out[b, 2048 * j:2048 * j + 2048, :] = np.asarray(res[core]["out"])
    return out
```

```python
import numpy as np
import concourse.bass as bass
import concourse.mybir as mybir
from concourse.bass_utils import run_bass_kernel_spmd
from contextlib import ExitStack

F32 = mybir.dt.float32
BF16 = mybir.dt.bfloat16
U32 = mybir.dt.uint32
I32 = mybir.dt.int32
AF = mybir.ActivationFunctionType
ALU = mybir.AluOpType

ENGS = ['pe', 'dve', 'act', 'pool', 'sp']
EPOCH = 20000
NSLOT = 12
SKIP_SELF = False


class Buf:
    __slots__ = ('w', 'r', 'name')

    def __init__(self, name=''):
        self.w = None
        self.r = {}
        self.name = name


class Ctx:
    def __init__(self, nc, stack):
        self.nc = nc
        self.stack = stack
        self.lists = {e: [] for e in ENGS}
        self.sems = {}
        self.cnt = {}
        self.epoch = {e: 0 for e in ENGS}
        self.seen = {e: {} for e in ENGS}
        self.dslot = {e: 0 for e in ENGS}
        self.dep = {e: 0 for e in ENGS}
        self.nsem = 0

    def _sem(self, key):
        if key not in self.sems:
            self.sems[key] = self.stack.enter_context(self.nc.semaphore(f"s{self.nsem}"))
            self.nsem += 1
            self.cnt[key] = 0
        return self.sems[key]

    def _issue(self, eng, fn, r, w, dma=False, skip_self=False, dinc=16):
        deps = {}

        def add(ev):
            if ev is None:
                return
            k, v = ev
            if deps.get(k, 0) < v:
                deps[k] = v
        for b in r:
            add(b.w)
        for b in w:
            add(b.w)
            for k, v in b.r.items():
                add((k, v))
        if dma:
            s = self.dslot[eng]
            self.dslot[eng] = (s + 1) % NSLOT
            key = ('d', eng, s, self.dep.get((eng, s), 0))
            self._sem(key)
            if self.cnt[key] + dinc > EPOCH:
                self.dep[(eng, s)] = self.dep.get((eng, s), 0) + 1
                old = key
                key = ('d', eng, s, self.dep[(eng, s)])
                self._sem(key)
                add((old, self.cnt[old]))
            else:
                add((key, self.cnt[key]))
            inc = dinc
        else:
            key = ('c', eng, self.epoch[eng])
            self._sem(key)
            if self.cnt[key] + 1 > EPOCH:
                self.epoch[eng] += 1
                key = ('c', eng, self.epoch[eng])
                self._sem(key)
            inc = 1
        waits = []
        seen = self.seen[eng]
        for k, v in deps.items():
            if v <= 0:
                continue
            if skip_self and k[0] == 'c' and k[1] == eng:
                continue
            if seen.get(k, 0) < v:
                seen[k] = v
                waits.append((k, v))
        self.cnt[key] += inc
        ev = (key, self.cnt[key])
        for b in r:
            if b.r.get(key, 0) < ev[1]:
                b.r[key] = ev[1]
        for b in w:
            b.w = ev
            b.r = {}
        self.lists[eng].append((waits, fn, key, inc))
        return ev

    def mm(self, fn, r, w):
        return self._issue('pe', fn, r, w, skip_self=True)

    def dve(self, fn, r, w):
        return self._issue('dve', fn, r, w, skip_self=SKIP_SELF)

    def act(self, fn, r, w):
        return self._issue('act', fn, r, w, skip_self=SKIP_SELF)

    def pool(self, fn, r, w):
        return self._issue('pool', fn, r, w)

    def dma(self, fn, r, w, q='sp'):
        return self._issue(q, fn, r, w, dma=True)

    def coll(self, fn, r, w):
        return self._issue('pool', fn, r, w, dma=True, dinc=1)

    def wait_all(self, eng, bufs):
        deps = {}
        for b in bufs:
            if b.w is not None:
                k, v = b.w
                deps[k] = max(deps.get(k, 0), v)
        self.lists[eng].append(([(k, v) for k, v in deps.items()], None, None, 0))

    def emit(self):
        nc = self.nc
        sems = self.sems
        lists = self.lists
        with nc.Block() as block:
            def run(e, name):
                for waits, fn, key, inc in lists[name]:
                    for k, v in waits:
                        e.wait_ge(sems[k], v)
                    if fn is not None:
                        ins = fn(e)
                        ins.then_inc(sems[key], inc)

            @block.tensor
            def _(e):
                run(e, 'pe')

            @block.vector
            def _(e):
                run(e, 'dve')

            @block.scalar
            def _(e):
                run(e, 'act')

            @block.gpsimd
            def _(e):
                run(e, 'pool')

            @block.sync
            def _(e):
                run(e, 'sp')
        self.lists = {e: [] for e in ENGS}

    def MM(self, out, lhsT, rhs, r, w, start=True, stop=True):
        return self.mm(lambda e: e.matmul(out, lhsT=lhsT, rhs=rhs, start=start, stop=stop), r, w)

    def TR(self, out, in_, ident, r, w):
        return self.mm(lambda e: e.transpose(out, in_, ident), r, w)

    def ACT(self, out, in_, func, r, w, **kw):
        return self.act(lambda e: e.activation(out=out, in_=in_, func=func, **kw), r, w)

    def _ve(self, eng):
        return {'dve': self.dve, 'pool': self.pool}[eng]

    def TT(self, out, in0, in1, op, r, w, eng='dve'):
        return self._ve(eng)(lambda e: e.tensor_tensor(out=out, in0=in0, in1=in1, op=op), r, w)

    def TS(self, out, in0, s1, s2, op0, op1, r, w, eng='dve'):
        if op1 is None:
            return self._ve(eng)(lambda e: e.tensor_scalar(out=out, in0=in0, scalar1=s1, scalar2=None, op0=op0), r, w)
        return self._ve(eng)(lambda e: e.tensor_scalar(out=out, in0=in0, scalar1=s1, scalar2=s2, op0=op0, op1=op1), r, w)

    def STT(self, out, in0, scalar, in1, op0, op1, r, w):
        return self.dve(lambda e: e.scalar_tensor_tensor(out=out, in0=in0, scalar=scalar, in1=in1, op0=op0, op1=op1), r, w)

    def CP(self, out, in_, r, w, eng='dve'):
        if eng == 'act':
            return self.act(lambda e: e.activation(out=out, in_=in_, func=AF.Copy), r, w)
        return self._ve(eng)(lambda e: e.tensor_copy(out=out, in_=in_), r, w)

    def RCP(self, out, in_, r, w):
        return self.dve(lambda e: e.reciprocal(out=out, in_=in_), r, w)

    def MSET(self, out, val, r, w, eng='dve'):
        return self._ve(eng)(lambda e: e.memset(out, val), r, w)

    def DMA(self, out, in_, r, w, q='sp'):
        return self.dma(lambda e: e.dma_start(out=out, in_=in_), r, w, q=q)


D = 2048
NTOK = 2112
BLKS = [(0, 64, 1)] + [(64 + 512 * i, 512, 0) for i in range(4)]
DIN = 9056
MCH = 71
NT = 8448
NTL = 66
NQ = 2048
SCALE = 192 ** -0.5
TB = 256
NBLK = NQ // TB
NEG = -1.0e30
GR = 27 * 128
RG = [[0, 1, 2, 3], [4, 5, 6, 7]]
BLK = [(512 * i, 512) for i in range(16)] + [(8192, 256)]
H_AB = 832 + 3072 + 1024


def pieces(t0, nb):
    out = []
    t = t0
    while t < t0 + nb:
        if t < 256:
            s, c = t // 64, t % 64
            ln = min(64 - c, t0 + nb - t)
        else:
            u = t - 256
            s, c = u // 2048, 64 + u % 2048
            ln = min(2048 - u % 2048, t0 + nb - t)
        out.append((t - t0, s, c, ln))
        t += ln
    return out


def phase_A(nc, k, T, stop_after=None):
    I = T['I']
    x_own = I("x_own", [NTOK, D]); cc = I("cc", [128, 16, 2]); w_mod = I("w_mod", [96, 128, 16, 128]); b_mod = I("b_mod", [128, 96])
    n1w = I("n1w", [128, 16]); w_in = I("w_in", [MCH, 128, 16, 128])
    XT, HW, MOD, G1i, G1fi = T['XT'], T['HW'], T['MOD'], T['G1i'], T['G1fi']
    with ExitStack() as st:
        sb = lambda n, s, dt=F32: st.enter_context(nc.sbuf_tensor("a_" + n, s, dt))
        ps = lambda n, s, dt=F32: st.enter_context(nc.psum_tensor("a_" + n, s, dt))
        b_c = Buf()
        idf = sb("idf", [128, 128]); oht = sb("oht", [128, 4])
        k.DMA(idf[:], T['ident'], [], [b_c]); k.DMA(oht[:], T['oh4'], [], [b_c])
        xtok = [sb(f"xtok{i}", [128, D]) for i in range(2)]; b_xtok = [Buf(), Buf()]
        xTs = [sb(f"xTs{i}", [128, 16, 128]) for i in range(2)]; b_xTs = [Buf(), Buf()]
        pT = [ps(f"pT{i}", [128, 4, 128]) for i in range(2)]; b_pT = [Buf(), Buf()]
        XTv = XT.rearrange("(c p) t -> p c t", p=128)
        tiles = [(0, 64)] + [(64 + 128 * i, 128) for i in range(16)]
        for ti, (r0, np_) in enumerate(tiles):
            i = ti % 2
            k.DMA(xtok[i][0:np_, :], x_own[r0:r0 + np_, :], [], [b_xtok[i]], q='sp' if i == 0 else 'pool')
            for c in range(16):
                pb = (c // 4) % 2
                k.TR(pT[pb][:, c % 4, 0:np_], xtok[i][0:np_, c * 128:(c + 1) * 128], idf[0:np_, 0:np_], [b_xtok[i], b_c], [b_pT[pb]])
                if c % 4 == 3:
                    k.CP(xTs[i][:, c - 3:c + 1, 0:np_], pT[pb][:, :, 0:np_], [b_pT[pb]], [b_xTs[i]], eng='act' if pb == 0 else 'dve')
            k.DMA(XTv[:, :, r0:r0 + np_], xTs[i][:, :, 0:np_], [b_xTs[i]], [T['b_XT']])
        cct = sb("cct", [128, 16, 2]); b_cct = Buf()
        bmt = sb("bmt", [128, 96]); b_bmt = Buf()
        n1t = sb("n1t", [128, 16]); b_n1t = Buf()
        modt = sb("modt", [128, 96, 2]); b_modt = Buf()
        scl = sb("scl", [128, 16, 2]); b_scl = Buf()
        ones = sb("ones", [128, 128]); b_ones = Buf()
        wst = [sb(f"wst{i}", [128, 16, 128]) for i in range(2)]; b_wst = [Buf(), Buf()]
        wbf = [sb(f"wbf{i}", [128, 16, 128], BF16) for i in range(2)]; b_wbf = [Buf(), Buf()]
        hT = sb("hT", [128, 16, NTOK], BF16); b_hT = Buf()
        xt = sb("xt", [128, 16, 512]); b_xt = Buf()
        sq = [sb(f"sq{i}", [128, 512]) for i in range(2)]; b_sq = [Buf(), Buf()]
        rstd = sb("rstd", [128, 512]); b_rstd = Buf()
        ot = [sb(f"ot{i}", [128, NTOK]) for i in range(2)]; b_ot = [Buf(), Buf()]
        ex = [sb(f"ex{i}", [128, NTOK], BF16) for i in range(2)]; b_ex = [Buf(), Buf()]
        exf = sb("exf", [128, NTOK]); b_exf = Buf()
        pmod = ps("pmod", [128, 512]); b_pmod = Buf()
        pg = [ps(f"pg{i}", [128, 512]) for i in range(5)]; b_pg = [Buf() for _ in range(5)]
        pss = pg[0]; b_pss = b_pg[0]
        k.DMA(cct[:], cc, [], [b_cct]); k.DMA(bmt[:], b_mod, [], [b_bmt]); k.DMA(n1t[:], n1w, [], [b_n1t])
        k.MSET(ones[:], 1.0, [], [b_ones])
        k.ACT(cct[:], cct[:], AF.Silu, [], [b_cct])
        for j in range(96):
            i = j % 2
            k.DMA(wst[i][:], w_mod[j], [], [b_wst[i]], q='sp' if j % 2 == 0 else 'pool')
            for c in range(16):
                k.MM(pmod[:, j * 2:j * 2 + 2], wst[i][:, c, :], cct[:, c, :], [b_wst[i], b_cct], [b_pmod], start=(c == 0), stop=(c == 15))
            k.TS(modt[:, j, :], pmod[:, j * 2:j * 2 + 2], bmt[:, j:j + 1], None, ALU.add, None, [b_pmod, b_bmt], [b_modt])
        modS = sb("modS", [128, 2, 96])
        for s_ in range(2):
            k.CP(modS[:, s_, :], modt[:, :, s_], [b_modt], [b_modt])
        k.DMA(MOD, modS[:], [b_modt], [T['b_MOD']])
        for s in range(2):
            k.STT(scl[:, :, s], modt[:, 16:32, s], 1.0, n1t[:], ALU.add, ALU.mult, [b_modt, b_n1t], [b_scl])
        for (t0, nb, s) in BLKS:
            k.DMA(xt[:, :, 0:nb], XTv[:, :, t0:t0 + nb], [T['b_XT']], [b_xt])
            for c in range(16):
                i = c % 2
                k.ACT(sq[i][:, 0:nb], xt[:, c, 0:nb], AF.Square, [b_xt], [b_sq[i]])
                k.MM(pss[:, 0:nb], ones[:], sq[i][:, 0:nb], [b_sq[i], b_ones], [b_pss], start=(c == 0), stop=(c == 15))
            k.TS(rstd[:, 0:nb], pss[:, 0:nb], 1.0 / D, 1e-6, ALU.mult, ALU.add, [b_pss], [b_rstd])
            k.ACT(rstd[:, 0:nb], rstd[:, 0:nb], AF.Sqrt, [], [b_rstd])
            k.RCP(rstd[:, 0:nb], rstd[:, 0:nb], [], [b_rstd])
            for c in range(16):
                i = c % 2
                k.TT(sq[i][:, 0:nb], xt[:, c, 0:nb], rstd[:, 0:nb], ALU.mult, [b_xt, b_rstd], [b_sq[i]])
                k.ACT(hT[:, c, t0:t0 + nb], sq[i][:, 0:nb], AF.Identity, [b_sq[i], b_scl, b_modt], [b_hT],
                      scale=scl[:, c, s:s + 1], bias=modt[:, c, s:s + 1])
        nex = 0
        for m in range(MCH):
            i = m % 2
            mw = min(128, DIN - m * 128)
            k.DMA(wbf[i][:], w_in[m], [], [b_wbf[i]], q='pool')
            for bi, (t0, nb, s) in enumerate(BLKS):
                for c in range(16):
                    k.MM(pg[bi][0:mw, 0:nb], wbf[i][:, c, 0:mw], hT[:, c, t0:t0 + nb], [b_wbf[i], b_hT], [b_pg[bi]],
                         start=(c == 0), stop=(c == 15))
                k.CP(ot[i][0:mw, t0:t0 + nb], pg[bi][0:mw, 0:nb], [b_pg[bi]], [b_ot[i]], eng='act' if bi % 2 == 0 else 'dve')
            k.DMA(HW[m * 128:m * 128 + mw, :], ot[i][0:mw, :], [b_ot[i]], [T['b_HW']])
            if 4 <= m <= 30:
                for s in range(4):
                    e_ = nex % 2; nex += 1
                    k.ACT(ex[e_][:], ot[i][:], AF.Identity, [b_ot[i], b_c], [b_ex[e_]], scale=oht[:, s:s + 1])
                    k.DMA(G1i[s * GR + (m - 4) * 128: s * GR + (m - 3) * 128, :], ex[e_][:], [b_ex[e_]], [T['b_G1i']],
                          q='sp' if e_ == 0 else 'pool')
            if m == 38:
                for s in range(4):
                    k.ACT(exf[64:96, :], ot[i][64:96, :], AF.Identity, [b_ot[i], b_c], [b_exf], scale=oht[64:96, s:s + 1])
                    k.DMA(G1fi[s * 32:(s + 1) * 32, :], exf[64:96, :], [b_exf], [T['b_G1fi']])
        k.wait_all('sp', [T['b_HW'], T['b_G1i'], T['b_G1fi'], T['b_MOD'], T['b_XT']])
        k.emit()


def allreduce_chunks(k, src, dst, rows, ch, r, w):
    for r0 in range(0, rows, ch):
        k.coll(lambda e, r0=r0: e.collective_compute("AllReduce", ALU.add, replica_groups=RG, ins=[src[r0:r0 + ch, :].opt()],
                                                     outs=[dst[r0:r0 + ch, :].opt()]), r, w)


def rope_tables():
    n = 8192
    rows = np.repeat(np.arange(n // 64, dtype=np.float32), 64)
    cols = np.tile(np.arange(64, dtype=np.float32), n // 64)
    inv = (np.float32(10000.0) ** (-np.arange(0, 32, 2, dtype=np.float32) / np.float32(32))).astype(np.float32)
    ar = rows[:, None] * inv; ac = cols[:, None] * inv
    cos = np.concatenate([np.cos(ar), np.cos(ar), np.cos(ac), np.cos(ac)], 1).T
    sin = np.concatenate([-np.sin(ar), np.sin(ar), -np.sin(ac), np.sin(ac)], 1).T
    return cos.astype(np.float32), sin.astype(np.float32)


PERM = np.concatenate([np.arange(16, 32), np.arange(0, 16), np.arange(48, 64), np.arange(32, 48)])


def gdn_consts():
    m = np.arange(128)[:, None]; t = np.arange(128)[None, :]
    f = lambda c: c.astype(np.float32)
    fw_ = [f(m <= t), f(m > t), -30000.0 * f(t > m), f(t < m)]
    bw_ = [f(m >= t), f(m < t), -30000.0 * f(t < m), f(t > m)]
    bm = []
    for lower in (True, False):
        for l in range(7):
            sz = 2 ** l
            same = ((m // (2 * sz)) == (t // (2 * sz))) & ((m // sz) != (t // sz))
            bm.append(f(same & ((m > t) if lower else (m < t))))
    return np.ascontiguousarray(np.stack(fw_ + bw_, 1)), np.eye(128, dtype=np.float32), np.ascontiguousarray(np.stack(bm, 1))


class StopBuild(Exception):
    pass


def phase_B(nc, k, T, do1=True, do2=True, stage=99):
    I = T['I']
    COS = I("COS", [64, NT]); SIN = I("SIN", [64, NT]); kvw = I("kvw", [128, 2]); wukv = I("wukv", [256, 512])
    convw = I("convw", [128, 6, 5]); alog = I("alog", [128, 4]); dtb = I("dtb", [128, 4]); masks = I("masks", [128, 8, 128])
    bmask = I("bmask", [128, 14, 128]); ident = T['ident']
    G1o, G1fo, GKi, GVi, GOi, KR = T['G1o'], T['G1fo'], T['GKi'], T['GVi'], T['GOi'], T['KR']
    G1v = G1o.rearrange("(s c p) t -> s p c t", s=4, p=128)
    bG1l = T['b_G1o']

    def g1b(r0, r1):
        return [bG1l[c_] for c_ in range(r0 // 512, (r1 - 1) // 512 + 1)]
    if True:
        b_out = Buf()
        with ExitStack() as st:
          if do1:
            sb = lambda n, s, dt=F32: st.enter_context(nc.sbuf_tensor("b_" + n, s, dt))
            ps = lambda n, s, dt=F32: st.enter_context(nc.psum_tensor("b_" + n, s, dt))
            ones = sb("ones1", [128, 128]); b_c = Buf()
            kvwt = sb("kvwt", [128, 2]); wst = sb("wukvs", [128, 2, 512]); wbf = sb("wukvb", [128, 2, 512], BF16)
            ckvn = sb("ckvn", [128, 2, NT], BF16); b_ckvn = Buf()
            xt = [sb(f"ckx{i}", [128, 2, 512], BF16) for i in range(2)]; b_xt = [Buf(), Buf()]
            sq = sb("cksq", [128, 512]); b_sq = Buf()
            rstd = sb("ckr", [128, 512]); b_rstd = Buf()
            kst = [sb(f"kst{i}", [128, 512], BF16) for i in range(2)]; b_kst = [Buf(), Buf()]
            vst = [sb(f"vst{i}", [128, 128], BF16) for i in range(2)]; b_vst = [Buf(), Buf()]
            rp = [sb(f"rp{i}", [64, 4, 512]) for i in range(2)]; b_rp = [Buf(), Buf()]
            rpo = [sb(f"rpo{i}", [64, 512], BF16) for i in range(2)]
            rpb = [sb(f"rpb{i}", [64, 2, 512], BF16) for i in range(2)]
            oht = sb("oht1", [128, 4]); k.DMA(oht[:], T["oh4"], [], [b_c]); nk = [0]; b_rpo = [Buf(), Buf()]
            pss = ps("pss1", [128, 512]); b_pss = Buf()
            pk = [ps(f"pk{i}", [128, 512]) for i in range(2)]; b_pk = [Buf(), Buf()]
            pvt = [ps(f"pvt{i}", [128, 4, 128]) for i in range(2)]; pv = [pvt[0][:, 0, :], pvt[1][:, 0, :]]; b_pv = [Buf(), Buf()]
            k.MSET(ones[:], 1.0, [], [b_c])
            k.DMA(kvwt[:], kvw, [], [b_c])
            k.DMA(wst[:], wukv.rearrange("(c p) n -> p c n", p=128), [], [b_c])
            k.CP(wbf[:], wst[:], [b_c], [b_c])
            for bi, (t0, nb) in enumerate(BLK):
                i = bi % 2
                for (off, s_, c0, ln) in pieces(t0, nb):
                    k.DMA(xt[i][:, :, off:off + ln], G1v[s_][:, 0:2, c0:c0 + ln], g1b(s_ * GR, s_ * GR + 256), [b_xt[i]])
                for c in range(2):
                    k.ACT(sq[:, 0:nb], xt[i][:, c, 0:nb], AF.Square, [b_xt[i]], [b_sq])
                    k.MM(pss[:, 0:nb], ones[:], sq[:, 0:nb], [b_sq, b_c], [b_pss], start=(c == 0), stop=(c == 1))
                k.TS(rstd[:, 0:nb], pss[:, 0:nb], 1.0 / 256, 1e-6, ALU.mult, ALU.add, [b_pss], [b_rstd])
                k.ACT(rstd[:, 0:nb], rstd[:, 0:nb], AF.Sqrt, [], [b_rstd])
                k.RCP(rstd[:, 0:nb], rstd[:, 0:nb], [], [b_rstd])
                for c in range(2):
                    k.STT(ckvn[:, c, t0:t0 + nb], xt[i][:, c, 0:nb], kvwt[:, c:c + 1], rstd[:, 0:nb], ALU.mult, ALU.mult,
                          [b_xt[i], b_rstd, b_c], [b_ckvn])
                for hl in range(2):
                    for c in range(2):
                        k.MM(pk[hl][:, 0:nb], wbf[:, c, hl * 256:hl * 256 + 128], ckvn[:, c, t0:t0 + nb], [b_c, b_ckvn], [b_pk[hl]],
                             start=(c == 0), stop=(c == 1))
                    for s_ in range(4):
                        e_ = nk[0] % 2; nk[0] += 1
                        k.ACT(kst[e_][:, 0:nb], pk[hl][:, 0:nb], AF.Identity, [b_pk[hl], b_c], [b_kst[e_]], scale=oht[:, s_:s_ + 1])
                        k.DMA(GKi[s_ * 256 + hl * 128:s_ * 256 + (hl + 1) * 128, t0:t0 + nb], kst[e_][:, 0:nb], [b_kst[e_]], [T['b_GKi']],
                              q='pool')
                for tt in range(nb // 128):
                    ti = t0 // 128 + tt
                    for hl in range(2):
                        for c in range(2):
                            k.MM(pv[hl], ckvn[:, c, ti * 128:(ti + 1) * 128], wbf[:, c, hl * 256 + 128:hl * 256 + 256],
                                 [b_c, b_ckvn], [b_pv[hl]], start=(c == 0), stop=(c == 1))
                        for s_ in range(4):
                            e_ = nk[0] % 2; nk[0] += 1
                            k.TS(vst[e_][:], pv[hl], oht[:, s_:s_ + 1], None, ALU.mult, None, [b_pv[hl], b_c], [b_vst[e_]])
                            r0_ = ((s_ * 2 + hl) * NTL + ti) * 128
                            k.DMA(GVi[r0_:r0_ + 128, :], vst[e_][:], [b_vst[e_]], [T['b_GVi']], q='pool')
                for (off, s_, c0, ln) in pieces(t0, nb):
                    k.DMA(rpb[i][:, 0, off:off + ln], G1o[s_ * GR + 256:s_ * GR + 320, c0:c0 + ln], g1b(s_ * GR + 256, s_ * GR + 320), [b_rp[i]])
                    for (d0, s0) in ((0, 16), (16, 0), (32, 48), (48, 32)):
                        k.DMA(rpb[i][d0:d0 + 16, 1, off:off + ln], G1o[s_ * GR + 256 + s0:s_ * GR + 256 + s0 + 16, c0:c0 + ln], g1b(s_ * GR + 256, s_ * GR + 320), [b_rp[i]],
                              q='pool')
                k.DMA(rp[i][:, 2, 0:nb], COS[:, t0:t0 + nb], [], [b_rp[i]])
                k.DMA(rp[i][:, 3, 0:nb], SIN[:, t0:t0 + nb], [], [b_rp[i]])
                k.TT(rp[i][:, 0, 0:nb], rpb[i][:, 0, 0:nb], rp[i][:, 2, 0:nb], ALU.mult, [], [b_rp[i]])
                k.TT(rp[i][:, 1, 0:nb], rpb[i][:, 1, 0:nb], rp[i][:, 3, 0:nb], ALU.mult, [], [b_rp[i]])
                k.TT(rpo[i][:, 0:nb], rp[i][:, 0, 0:nb], rp[i][:, 1, 0:nb], ALU.add, [b_rp[i]], [b_rpo[i]], eng='pool')
                k.DMA(KR[:, t0:t0 + nb], rpo[i][:, 0:nb], [b_rpo[i]], [T['b_KR']])
            k.wait_all('sp', [T['b_KR'], T['b_GKi'], T['b_GVi']])
            k.emit()
        with ExitStack() as st:
          if do2:
            sb = lambda n, s, dt=F32: st.enter_context(nc.sbuf_tensor("b_" + n, s, dt))
            ps = lambda n, s, dt=F32: st.enter_context(nc.psum_tensor("b_" + n, s, dt))
            b_c = Buf()
            ones = sb("ones2", [128, 128]); onesb = sb("ones2b", [128, 128], BF16)
            idf = sb("idf", [128, 128]); idb = sb("idb", [128, 128], BF16)
            mk = sb("mk", [128, 8, 128]); abt = sb("abt", [128, NTL, 8]); cwt = sb("cwt", [128, 6, 5])
            bmk = sb("bmk", [128, 14, 128]); II32 = sb("II32", [128, 2, 128]); IIb = sb("IIb", [128, 2, 128], BF16)
            alt = sb("alt", [128, 4]); dtt = sb("dtt", [128, 4]); oht = sb("oht2", [128, 4])
            k.MSET(ones[:], 1.0, [], [b_c]); k.MSET(onesb[:], 1.0, [], [b_c])
            for dst, src in [(idf, ident), (mk, masks), (cwt, convw), (alt, alog), (dtt, dtb), (bmk, bmask), (oht, T['oh4'])]:
                k.DMA(dst[:], src, [], [b_c])
            k.CP(idb[:], idf[:], [b_c], [b_c])
            for q_ in range(2):
                k.CP(II32[:, q_, :], idf[:], [b_c], [b_c]); k.CP(IIb[:, q_, :], idf[:], [b_c], [b_c])
            k.ACT(alt[:], alt[:], AF.Exp, [b_c], [b_c])
            pre = sb("pre", [128, NT]); b_pre = Buf()
            cvb = sb("cvb", [128, NT]); b_cv = Buf()
            qkb = [sb(f"qkb{g}", [128, NT], BF16) for g in range(3)]; b_qk = [Buf() for _ in range(3)]
            sqb = sb("sqb", [128, 512], BF16); b_sqb = Buf()
            rs2 = sb("rs2", [128, 512]); b_rs2 = Buf()
            pss = ps("pss2", [128, 512]); b_pss = Buf()
            gt = sb("gt", [128, NTL]); bet = sb("bet", [128, NTL]); egc = sb("egc", [128, NTL]); erest = sb("erest", [128, NTL])
            egl = sb("egl", [128, NTL]); nc1 = sb("nc1", [128, NTL]); b_sc = Buf()
            psc = ps("psc", [128, 4, 128]); b_psc = Buf()
            b_Tg = Buf()
            b_Dm = Buf()
            b_MA = Buf()
            b_TR = Buf()
            b_Cst = Buf()
            b_DE = Buf(); b_DEb = Buf()
            b_Gb = Buf()
            b_bk = Buf()
            rr = sb("rr", [128, 128], BF16); b_rr = Buf()
            oq = sb("oq", [128, 128]); b_oq = Buf()
            vn = sb("vn", [128, 128], BF16); b_vn = Buf()
            S = sb("S", [128, 128]); Sb = sb("Sb", [128, 128], BF16); b_S = Buf(); b_S32 = Buf()
            ost = [sb(f"ost{i}", [128, 128]) for i in range(2)]; b_ost = [Buf(), Buf()]
            psA = ps("psA", [128, 4, 128]); b_psA = Buf()
            psT = ps("psT", [128, 8, 128], BF16); b_psT = Buf()
            psB = ps("psB", [128, 4, 128]); b_psB = Buf()
            psS = ps("psS", [128, 4, 128]); b_psS = Buf()
            psUt = ps("psU", [128, 4, 128]); psU = psUt[:, 0, :]; b_psU = Buf()
            psO = ps("psO", [128, 4, 128]); b_psO = Buf()
            cand = [sb(f"cand{i}", [128, NT], BF16) for i in range(2)]; b_cand = [Buf(), Buf()]
            oex = [sb(f"oex{i}", [128, 128], BF16) for i in range(2)]; b_oex = [Buf(), Buf()]
            nq = [0]
            ab8 = pre[0:8, :]; abc = cvb[0:8, :]; b_ab8 = b_pre; b_abc = b_cv
            G1fv = G1fo.rearrange("(s k h) t -> s k h t", s=4, k=4)
            for jc in range(4):
                for (off, s_, c0, ln) in pieces(0, NT):
                    for kind in range(4):
                        k.DMA(abc[kind * 2:kind * 2 + 2, off:off + ln], G1fv[s_][kind, 2 * jc:2 * jc + 2, c0:c0 + ln], [T['b_G1fo']], [b_abc],
                              q='sp' if kind % 2 == 0 else 'pool')
                if jc == 0:
                    k.TS(ab8, abc, oht[0:8, 0:1], None, ALU.mult, None, [b_abc, b_c], [b_ab8])
                else:
                    k.STT(ab8, abc, oht[0:8, jc:jc + 1], ab8, ALU.mult, ALU.add, [b_abc, b_c], [b_ab8])
            for half in range(2):
                for tq in range(33):
                    ti = half * 33 + tq
                    k.TR(psO[:, :, :].rearrange("p a b -> p (a b)")[:, tq * 8:tq * 8 + 8], ab8[0:8, ti * 128:(ti + 1) * 128], idf[0:8, 0:8],
                         [b_ab8, b_c], [b_psO])
                k.CP(abt[:, half * 33:(half + 1) * 33, :], psO[:, :, :].rearrange("p a b -> p (a b)")[:, 0:264].rearrange("p (a b) -> p a b", b=8),
                     [b_psO], [b_c])
            oacc = pre[:, 0:8192].rearrange("p (a b) -> p a b", b=128)
            psA2 = pss[:].rearrange("p (a b) -> p a b", b=128)
            psB2 = psc[:].rearrange("p (a b) c -> p a b c", b=2)
            Tg2 = sb("Tg2", [128, 2, 128]); Dm2 = sb("Dm2", [128, 2, 128]); DmS2 = sb("DmS2", [128, 2, 128])
            Mm2 = sb("Mm2", [128, 2, 128], BF16); At2 = sb("At2", [128, 2, 128], BF16); TRt2 = sb("TRt2", [128, 8, 128], BF16)
            Cst2 = sb("Cst2", [128, 2, 7, 128], BF16); DE32_2 = sb("DE32_2", [128, 2, 2, 128]); DEb2 = sb("DEb2", [128, 2, 2, 128], BF16)
            Gb2 = sb("Gb2", [128, 2, 128], BF16); bv2 = sb("bv2", [128, 2, 128]); kdec2 = sb("kdec2", [128, 2, 128], BF16)
            II32_2 = sb("II32_2", [128, 2, 2, 128]); IIb_2 = sb("IIb_2", [128, 2, 2, 128], BF16)
            for a_ in range(2):
                for q_ in range(2):
                    k.CP(II32_2[:, a_, q_, :], idf[:], [b_c], [b_c]); k.CP(IIb_2[:, a_, q_, :], idf[:], [b_c], [b_c])
            try:
             for hl in range(2):
                for g in range(3):
                    for jc in range(4):
                        ci = nq[0] % 2; nq[0] += 1
                        rb = 320 + g * 1024 + (2 * jc + hl) * 128
                        for pi_, (off, s_, c0, ln) in enumerate(pieces(0, NT)):
                            k.DMA(cand[ci][:, off:off + ln], G1o[s_ * GR + rb:s_ * GR + rb + 128, c0:c0 + ln], g1b(s_ * GR + rb, s_ * GR + rb + 128), [b_cand[ci]],
                                  q='sp' if pi_ % 2 == 0 else 'pool')
                        if jc == 0:
                            k.TS(pre[:], cand[ci][:], oht[:, 0:1], None, ALU.mult, None, [b_cand[ci], b_c], [b_pre])
                        else:
                            k.STT(pre[:], cand[ci][:], oht[:, jc:jc + 1], pre[:], ALU.mult, ALU.add, [b_cand[ci], b_c], [b_pre])
                    for (lo, hi) in [(0, 256), (256, NT)]:
                        k.TS(cvb[:, lo:hi], pre[:, lo:hi], cwt[:, hl * 3 + g, 2:3], None, ALU.mult, None, [b_pre, b_c], [b_cv])
                        for s_ in (-2, -1, 1, 2):
                            a_, b_ = (lo - s_, hi) if s_ < 0 else (lo, hi - s_)
                            k.STT(cvb[:, a_:b_], pre[:, a_ + s_:b_ + s_], cwt[:, hl * 3 + g, s_ + 2:s_ + 3], cvb[:, a_:b_],
                                  ALU.mult, ALU.add, [b_pre, b_c], [b_cv])
                    k.ACT(cvb[:], cvb[:], AF.Silu, [], [b_cv])
                    if g == 2:
                        k.CP(qkb[2][:], cvb[:], [b_cv], [b_qk[2]], eng='pool')
                    else:
                        for (t0, nb) in BLK:
                            k.ACT(sqb[:, 0:nb], cvb[:, t0:t0 + nb], AF.Square, [b_cv], [b_sqb])
                            k.MM(pss[:, 0:nb], onesb[:], sqb[:, 0:nb], [b_sqb, b_c], [b_pss])
                            k.TS(rs2[:, 0:nb], pss[:, 0:nb], 1e-6, None, ALU.add, None, [b_pss], [b_rs2])
                            k.ACT(rs2[:, 0:nb], rs2[:, 0:nb], AF.Sqrt, [], [b_rs2])
                            k.RCP(rs2[:, 0:nb], rs2[:, 0:nb], [], [b_rs2])
                            k.STT(qkb[g][:, t0:t0 + nb], cvb[:, t0:t0 + nb], (128 ** -0.5) if g == 0 else 1.0, rs2[:, 0:nb],
                                  ALU.mult, ALU.mult, [b_cv, b_rs2], [b_qk[g]])
                if stage == 1:
                    raise StopBuild()
                qT, kT, vT = qkb
                b_q, b_k, b_v = b_qk
                for d in range(2):
                    Tm, Um, Ng, Ms = (mk[:, 4 * d + j, :] for j in range(4))
                    ca = (0 + d) * 2 + hl
                    cb = (2 + d) * 2 + hl
                    col = d * 2 + hl
                    k.ACT(gt[:], abt[:, :, ca], AF.Exp, [b_c], [b_sc], bias=dtt[:, col:col + 1])
                    k.ACT(gt[:], gt[:], AF.Ln, [], [b_sc], bias=1.0)
                    k.TS(gt[:], gt[:], alt[:, col:col + 1], -1.0, ALU.mult, ALU.mult, [b_c], [b_sc])
                    k.ACT(bet[:], abt[:, :, cb], AF.Sigmoid, [b_c], [b_sc])
                    k.MM(psc[:, 0, 0:NTL], Tm, gt[:], [b_sc, b_c], [b_psc])
                    k.MM(psc[:, 1, 0:NTL], Um, gt[:], [b_sc, b_c], [b_psc])
                    k.MM(psc[:, 2, 0:NTL], ones[:], gt[:], [b_sc, b_c], [b_psc])
                    k.ACT(egc[:], psc[:, 0, 0:NTL], AF.Exp, [b_psc], [b_sc])
                    k.ACT(erest[:], psc[:, 1, 0:NTL], AF.Exp, [b_psc], [b_sc])
                    k.ACT(egl[:], psc[:, 2, 0:NTL], AF.Exp, [b_psc], [b_sc])
                    k.STT(nc1[:], bet[:], -1.0, egc[:], ALU.mult, ALU.mult, [], [b_sc])
                    k.MSET(S[:], 0.0, [], [b_S32]); k.MSET(Sb[:], 0.0, [], [b_S])
                    if stage == 2:
                        raise StopBuild()
                    order = list(range(NTL)) if d == 0 else [1, 0] + list(range(NTL - 1, 1, -1))
                    pairs = [(order[2 * n_], order[2 * n_ + 1]) for n_ in range(NTL // 2)]
                    for tis in pairs:
                        sls = [slice(t_ * 128, (t_ + 1) * 128) for t_ in tis]
                        for s_, ti in enumerate(tis):
                            k.TS(Tg2[:, s_, :], Tm, gt[:, ti:ti + 1], None, ALU.mult, None, [b_sc, b_c], [b_Tg], eng='pool')
                        for s_ in range(2):
                            k.MM(psA[:, s_, :], Tg2[:, s_, :], Um, [b_Tg, b_c], [b_psA], start=True, stop=False)
                            k.MM(psA[:, s_, :], idf[:], Ng, [b_c], [b_psA], start=False, stop=True)
                        for s_ in range(2):
                            k.MM(psA[:, 2 + s_, :], kT[:, sls[s_]], kT[:, sls[s_]], [b_k], [b_psA])
                        for s_ in range(2):
                            k.MM(psA2[:, s_, :], qT[:, sls[s_]], kT[:, sls[s_]], [b_q, b_k], [b_pss])
                        k.ACT(Dm2[:], psA[:, 0:2, :], AF.Exp, [b_psA], [b_Dm])
                        k.TT(DmS2[:], Dm2[:], Ms.unsqueeze(1).broadcast_to([128, 2, 128]), ALU.mult, [b_c], [b_Dm], eng='pool')
                        for s_, ti in enumerate(tis):
                            k.STT(Mm2[:, s_, :], psA[:, 2 + s_, :], bet[:, ti:ti + 1], DmS2[:, s_, :], ALU.mult, ALU.mult,
                                  [b_psA, b_Dm, b_sc], [b_MA])
                        k.TT(At2[:], psA2[:, 0:2, :], Dm2[:], ALU.mult, [b_pss, b_Dm], [b_MA])
                        for s_ in range(2):
                            k.TR(psT[:, 4 * s_ + 0, :], Mm2[:, s_, :], idb[:], [b_MA, b_c], [b_psT])
                            k.TR(psT[:, 4 * s_ + 1, :], At2[:, s_, :], idb[:], [b_MA, b_c], [b_psT])
                            k.TR(psT[:, 4 * s_ + 2, :], kT[:, sls[s_]], idb[:], [b_k, b_c], [b_psT])
                            k.TR(psT[:, 4 * s_ + 3, :], vT[:, sls[s_]], idb[:], [b_v, b_c], [b_psT])
                        k.CP(TRt2[:], psT[:], [b_psT], [b_TR], eng='act')
                        for s_, ti in enumerate(tis):
                            k.TS(bv2[:, s_, :], TRt2[:, 4 * s_ + 3, :], bet[:, ti:ti + 1], None, ALU.mult, None, [b_TR, b_sc], [b_bk], eng='pool')
                            k.TS(kdec2[:, s_, :], TRt2[:, 4 * s_ + 2, :], erest[:, ti:ti + 1], None, ALU.mult, None, [b_TR, b_sc], [b_bk], eng='pool')
                        k.TT(Cst2[:], Mm2[:].unsqueeze(2).broadcast_to([128, 2, 7, 128]),
                             bmk[:, 7 * d:7 * d + 7, :].unsqueeze(1).broadcast_to([128, 2, 7, 128]), ALU.mult, [b_MA, b_c], [b_Cst])
                        k.CP(DE32_2[:], II32_2[:], [b_c], [b_DE], eng='pool')
                        k.CP(DEb2[:], IIb_2[:], [b_c], [b_DEb], eng='pool')
                        for lv in range(7):
                            for s_ in range(2):
                                k.MM(psB[:, s_, :], Cst2[:, s_, lv, :], DEb2[:, s_, 0, :], [b_Cst, b_DEb], [b_psB])
                            k.CP(Gb2[:], psB[:, 0:2, :], [b_psB], [b_Gb], eng='act')
                            for s_ in range(2):
                                k.MM(psB2[:, s_, 0, :], DEb2[:, s_, 1, :], Gb2[:, s_, :], [b_DEb, b_Gb], [b_psc])
                                if lv < 6:
                                    k.MM(psB2[:, s_, 1, :], Gb2[:, s_, :], DEb2[:, s_, 1, :], [b_DEb, b_Gb], [b_psc])
                            if lv < 6:
                                k.TT(DE32_2[:], DE32_2[:], psB2[:], ALU.subtract, [b_psc], [b_DE])
                                k.CP(DEb2[:], DE32_2[:], [b_DE], [b_DEb], eng='act')
                            else:
                                k.TT(DEb2[:, :, 0, :], DE32_2[:, :, 0, :], psB2[:, :, 0, :], ALU.subtract, [b_psc, b_DE], [b_DEb])
                        cur = 0
                        b_XY = [b_DEb]
                        for s_, ti in enumerate(tis):
                            sl = sls[s_]
                            lat = ti >= 2
                            AinvT = DEb2[:, s_, 0, :]
                            k.MM(psS[:, 0, :], kT[:, sl], Sb[:], [b_k, b_S], [b_psS])
                            if lat:
                                k.MM(psS[:, 1, :], qT[:, sl], Sb[:], [b_q, b_S], [b_psS])
                                if True:
                                    k.TS(oq[:], psS[:, 1, :], egc[:, ti:ti + 1], None, ALU.mult, None, [b_psS, b_sc], [b_oq])
                                else:
                                    k.ACT(oq[:], psS[:, 1, :], AF.Copy, [b_psS, b_sc], [b_oq], scale=egc[:, ti:ti + 1])
                                if d == 1:
                                    k.TT(oq[:], oq[:], oacc[:, ti - 2, :], ALU.add, [b_pre], [b_oq], eng='pool')
                            k.STT(rr[:], psS[:, 0, :], nc1[:, ti:ti + 1], bv2[:, s_, :], ALU.mult, ALU.add, [b_psS, b_sc, b_bk], [b_rr])
                            k.MM(psS[:, 2, :], AinvT, rr[:], [b_XY[cur], b_rr], [b_psS])
                            k.CP(vn[:], psS[:, 2, :], [b_psS], [b_vn], eng='act')
                            k.MM(psU, kdec2[:, s_, :], vn[:], [b_bk, b_vn], [b_psU])
                            if lat:
                                k.MM(psS[:, 3, :], TRt2[:, 4 * s_ + 1, :], vn[:], [b_TR, b_vn], [b_psS])
                                if d == 0:
                                    if False:
                                        k.TT(ost[0][:], psS[:, 3, :], oq[:], ALU.add, [b_psS, b_oq], [b_ost[0]])
                                    else:
                                        k.TT(oacc[:, ti - 2, :], psS[:, 3, :], oq[:], ALU.add, [b_psS, b_oq], [b_pre])
                                else:
                                    i = ti % 2
                                    k.TT(ost[i][:], psS[:, 3, :], oq[:], ALU.add, [b_psS, b_oq], [b_ost[i]])
                                    k.TR(psO[:, 0, :], ost[i][:], idf[:], [b_ost[i], b_c], [b_psO])
                                    for s4 in range(4):
                                        e_ = nq[0] % 2; nq[0] += 1
                                        k.ACT(oex[e_][:], psO[:, 0, :], AF.Identity, [b_psO, b_c], [b_oex[e_]], scale=oht[:, s4:s4 + 1])
                                        k.DMA(GOi[s4 * 256 + hl * 128:s4 * 256 + (hl + 1) * 128, (ti - 2) * 128:(ti - 1) * 128], oex[e_][:],
                                              [b_oex[e_]], [T['b_GOi']], q='sp' if e_ == 0 else 'pool')
                            k.STT(Sb[:], S[:], egl[:, ti:ti + 1], psU, ALU.mult, ALU.add, [b_psU, b_sc, b_S32], [b_S])
                            k.STT(S[:], S[:], egl[:, ti:ti + 1], psU, ALU.mult, ALU.add, [b_psU, b_sc], [b_S32])
                            if stage == 3 or (stage == 4 and ti == 3) or (stage >= 100 and ti == stage - 100):
                                raise StopBuild()
            except StopBuild:
                pass
            k.wait_all('sp', [T['b_GOi'], b_S, b_S32, b_sc, b_qk[0], b_qk[1], b_qk[2]])
            k.emit()
    return nc


def phase_C(nc, k, T, st0):
    I = T['I']
    qnw = I("qnw", [128, 4]); wuqn = I("wuqn", [8, 512, 128]); wuqr = I("wuqr", [8, 512, 64]); wuqp = I("wuqp", [8, 512, 64])
    COSq = I("COSq", [64, NQ]); SINq = I("SINq", [64, NQ]); dnw = I("dnw", [128, 1])
    wa = I("wa", [16, 128, 8, 128]); wb = I("wb", [16, 128, 8, 128]); wo = I("wo", [16, 128, 16, 128])
    HW, XT, MOD, GKo, GVo, GOo, KR, X1T = T['HW'], T['XT'], T['MOD'], T['GKo'], T['GVo'], T['GOo'], T['KR'], T['X1T']
    bHW = T['b_HW']
    if True:
        yaT = st0.enter_context(nc.sbuf_tensor("c_yaT", [128, 8, NQ], BF16)); b_ya = Buf()
        with ExitStack() as st:
            sb = lambda n, s, dt=F32: st.enter_context(nc.sbuf_tensor("c_" + n, s, dt))
            ps = lambda n, s, dt=F32: st.enter_context(nc.psum_tensor("c_" + n, s, dt))
            b_c = Buf()
            ones = sb("ones", [128, 128]); onesb = sb("onesb", [128, 128], BF16)
            qnwt = sb("qnwt", [128, 4]); cosq = sb("cosq", [64, NQ]); sinq = sb("sinq", [64, NQ]); krs = sb("krs", [64, NT], BF16)
            k.MSET(ones[:], 1.0, [], [b_c]); k.MSET(onesb[:], 1.0, [], [b_c])
            for dst, src in [(qnwt, qnw), (cosq, COSq), (sinq, SINq)]:
                k.DMA(dst[:], src, [], [b_c])
            k.DMA(krs[:], KR, [T['b_KR']], [b_c])
            cqn = sb("cqn", [128, 4, NQ], BF16); b_cqn = Buf()
            xt = sb("cqx", [128, 4, 512]); b_xt = Buf()
            sq = sb("cqsq", [128, 512]); b_sq = Buf()
            rstd = sb("cqr", [128, 512]); b_rstd = Buf()
            pss = ps("pss", [128, 512]); b_pss = Buf()
            cv = HW[0:512, :].rearrange("(c p) t -> p c t", p=128)
            for qb in range(4):
                t0 = qb * 512
                k.DMA(xt[:], cv[:, :, 64 + t0:64 + t0 + 512], [bHW], [b_xt])
                for c in range(4):
                    k.ACT(sq[:], xt[:, c, :], AF.Square, [b_xt], [b_sq])
                    k.MM(pss[:], ones[:], sq[:], [b_sq, b_c], [b_pss], start=(c == 0), stop=(c == 3))
                k.TS(rstd[:], pss[:], 1.0 / 512, 1e-6, ALU.mult, ALU.add, [b_pss], [b_rstd])
                k.ACT(rstd[:], rstd[:], AF.Sqrt, [], [b_rstd])
                k.RCP(rstd[:], rstd[:], [], [b_rstd])
                for c in range(4):
                    k.STT(cqn[:, c, t0:t0 + 512], xt[:, c, :], qnwt[:, c:c + 1], rstd[:], ALU.mult, ALU.mult,
                          [b_xt, b_rstd, b_c], [b_cqn])
            Kh = [sb(f"Kh{i}", [128, NT], BF16) for i in range(2)]; Vh = [sb(f"Vh{i}", [128, NTL, 128], BF16) for i in range(2)]
            b_kv = [Buf(), Buf()]
            wst = sb("wst", [128, 4, 256]); wbf = [sb(f"wbf{i}", [128, 4, 256], BF16) for i in range(2)]; b_wst = Buf(); b_w = [Buf(), Buf()]
            qn = [sb(f"qn{i}", [128, 512], BF16) for i in range(2)]; qr = [sb(f"qr{i}", [64, 512], BF16) for i in range(2)]
            b_q = [Buf(), Buf()]
            rt = sb("rt", [64, 2, 512]); b_rt = Buf()
            PT = [sb(f"PT{i}", [128, 512], BF16) for i in range(3)]; b_PT = [Buf() for _ in range(3)]
            rz = sb("rz", [128, 512]); b_rz = Buf()
            zacc = [sb(f"zacc{i}", [128, 2, 512]) for i in range(2)]; b_zacc = [[Buf(), Buf()], [Buf(), Buf()]]
            pS = [ps(f"pS{i}", [128, 512]) for i in range(2)]; b_pS = [Buf(), Buf()]
            pO = ps("pO", [128, 512]); b_pO = Buf(); pZ = ps("pZ", [128, 512]); b_pZ = Buf()
            pQ = [ps(f"pQ{i}", [128, 512]) for i in range(3)]; b_pQ = [Buf() for _ in range(3)]
            it = 0
            for h in range(8):
                i = h % 2
                s_, hl_ = h // 2, h % 2
                k.DMA(Kh[i][:], GKo[s_ * 256 + hl_ * 128:s_ * 256 + (hl_ + 1) * 128, :], [T['b_GKo'][h]], [b_kv[i]], q='sp')
                vr0 = (s_ * 2 + hl_) * NTL * 128
                k.DMA(Vh[i][:], GVo[vr0:vr0 + NTL * 128, :].rearrange("(n p) d -> p n d", p=128), [T['b_GVo'][h]], [b_kv[i]], q='pool')
                for off, src, wd in [(0, wuqn, 128), (128, wuqr, 64), (192, wuqp, 64)]:
                    k.DMA(wst[:, :, off:off + wd], src[h].rearrange("(c p) n -> p c n", p=128), [], [b_wst])
                k.CP(wbf[i][:], wst[:], [b_wst], [b_w[i]], eng='pool')
                for qb in range(4):
                    t0 = qb * 512
                    j = (h * 4 + qb) % 2
                    for pi, (off, wd) in enumerate([(0, 128), (128, 64), (192, 64)]):
                        for c in range(4):
                            k.MM(pQ[pi][0:wd, :], wbf[i][:, c, off:off + wd], cqn[:, c, t0:t0 + 512], [b_w[i], b_cqn], [b_pQ[pi]],
                                 start=(c == 0), stop=(c == 3))
                    k.CP(qn[j][:], pQ[0][:], [b_pQ[0]], [b_q[j]], eng='act')
                    k.TT(rt[:, 0, :], pQ[1][0:64, :], cosq[:, t0:t0 + 512], ALU.mult, [b_pQ[1], b_c], [b_rt])
                    k.TT(rt[:, 1, :], pQ[2][0:64, :], sinq[:, t0:t0 + 512], ALU.mult, [b_pQ[2], b_c], [b_rt])
                    k.TT(qr[j][:], rt[:, 0, :], rt[:, 1, :], ALU.add, [b_rt], [b_q[j]], eng='pool')
                    for kt in range(NTL):
                        si = it % 2; pi = it % 3; it += 1
                        ks = slice(kt * 128, (kt + 1) * 128)
                        k.MM(pS[si][:], Kh[i][:, ks], qn[j][:], [b_kv[i], b_q[j]], [b_pS[si]], start=True, stop=False)
                        k.MM(pS[si][:], krs[:, ks], qr[j][:], [b_c, b_q[j]], [b_pS[si]], start=False, stop=True)
                        k.ACT(PT[pi][:], pS[si][:], AF.Exp, [b_pS[si]], [b_PT[pi]], scale=SCALE)
                        k.MM(pO[:], Vh[i][:, kt, :], PT[pi][:], [b_kv[i], b_PT[pi]], [b_pO], start=(kt == 0), stop=(kt == NTL - 1))
                        zp = kt % 2
                        if kt < 2:
                            k.CP(zacc[j][:, zp, :], PT[pi][:], [b_PT[pi]], [b_zacc[j][zp]], eng='dve')
                        else:
                            k.TT(zacc[j][:, zp, :], zacc[j][:, zp, :], PT[pi][:], ALU.add, [b_PT[pi]], [b_zacc[j][zp]], eng='dve')
                    k.MM(pZ[:], ones[:], zacc[j][:, 0, :], [b_c, b_zacc[j][0]], [b_pZ], start=True, stop=False)
                    k.MM(pZ[:], ones[:], zacc[j][:, 1, :], [b_c, b_zacc[j][1]], [b_pZ], start=False, stop=True)
                    k.RCP(rz[:], pZ[:], [b_pZ], [b_rz])
                    k.TT(yaT[:, h, t0:t0 + 512], pO[:], rz[:], ALU.mult, [b_pO, b_rz], [b_ya])
            k.emit()
        with ExitStack() as st:
            sb = lambda n, s, dt=F32: st.enter_context(nc.sbuf_tensor("c_" + n, s, dt))
            ps = lambda n, s, dt=F32: st.enter_context(nc.psum_tensor("c_" + n, s, dt))
            b_c = Buf()
            ones = sb("ones3", [128, 128]); dnwt = sb("dnwt", [128, 1]); g1t = sb("g1t", [128, 16]); oht = sb("oht3", [128, 4])
            k.MSET(ones[:], 1.0, [], [b_c]); k.DMA(dnwt[:], dnw, [], [b_c]); k.DMA(g1t[:], MOD[:, 0, 32:48], [T["b_MOD"]], [b_c])
            k.DMA(oht[:], T["oh4"], [], [b_c])
            ocd = [sb(f"ocd{i}", [128, 512], BF16) for i in range(2)]; b_ocd = [Buf(), Buf()]; no = [0]
            wso = [sb(f"wso{i}", [128, 16, 128]) for i in range(2)]; wbo = [sb(f"wbo{i}", [128, 16, 128], BF16) for i in range(2)]
            b_wso = [Buf(), Buf()]; b_wbo = [Buf(), Buf()]
            ybT = sb("ybT", [128, 8, 512], BF16); b_yb = Buf()
            mg = sb("mg", [128, 16, 512], BF16); b_mg = Buf()
            os_ = [sb(f"os{i}", [128, 512]) for i in range(2)]; zz = [sb(f"zz{i}", [128, 512]) for i in range(2)]; b_oz = [Buf(), Buf()]
            sq = sb("sq3", [128, 512]); b_sq = Buf(); rs = sb("rs3", [128, 512]); b_rs = Buf()
            wst = [sb(f"wst3{i}", [128, 8, 256]) for i in range(2)]; wbf = [sb(f"wbf3{i}", [128, 8, 256], BF16) for i in range(2)]
            b_wst = [Buf(), Buf()]; b_wbf = [Buf(), Buf()]
            gt = [sb(f"gt3{i}", [128, 2, 512]) for i in range(2)]; b_gt = [Buf(), Buf()]
            t1 = sb("t13", [128, 512]); t2 = sb("t23", [128, 512]); b_t = Buf()
            xs = [sb(f"xs{i}", [128, 512]) for i in range(2)]; b_xs = [Buf(), Buf()]
            ys = [sb(f"ys{i}", [128, 512]) for i in range(2)]; b_ys = [Buf(), Buf()]
            pss = ps("pss3", [128, 512]); b_pss = Buf()
            pA = ps("pA", [128, 512]); pB = ps("pB", [128, 512]); b_pA = Buf(); b_pB = Buf()
            pY = [ps(f"pY{i}", [128, 512]) for i in range(2)]; b_pY = [Buf(), Buf()]
            zv = HW[3904:4928, :].rearrange("(c p) t -> p c t", p=128)
            gav = HW[4960:7008, :].rearrange("(c p) t -> p c t", p=128); gbv = HW[7008:9056, :].rearrange("(c p) t -> p c t", p=128)
            XTv = XT.rearrange("(c p) t -> p c t", p=128); X1v = X1T.rearrange("(c p) t -> p c t", p=128)
            for qb in range(4):
                t0 = qb * 512
                for hh in range(8):
                    i = hh % 2
                    for jc in range(4):
                        ci = no[0] % 2; no[0] += 1
                        orow = (hh // 2) * 256 + (hh % 2) * 128
                        k.DMA(ocd[ci][:], GOo[orow:orow + 128, jc * 2048 + t0:jc * 2048 + t0 + 512], [T['b_GOo'][hh]], [b_ocd[ci]], q='sp')
                        if jc == 0:
                            k.TS(os_[i][:], ocd[ci][:], oht[:, 0:1], None, ALU.mult, None, [b_ocd[ci], b_c], [b_oz[i]])
                        else:
                            k.STT(os_[i][:], ocd[ci][:], oht[:, jc:jc + 1], os_[i][:], ALU.mult, ALU.add, [b_ocd[ci], b_c], [b_oz[i]])
                    k.DMA(zz[i][:], zv[:, hh, 64 + t0:64 + t0 + 512], [bHW], [b_oz[i]], q='pool')
                    k.ACT(sq[:], os_[i][:], AF.Square, [b_oz[i]], [b_sq])
                    k.MM(pss[:], ones[:], sq[:], [b_sq, b_c], [b_pss])
                    k.TS(rs[:], pss[:], 1.0 / 128, 1e-6, ALU.mult, ALU.add, [b_pss], [b_rs])
                    k.ACT(rs[:], rs[:], AF.Sqrt, [], [b_rs])
                    k.RCP(rs[:], rs[:], [], [b_rs])
                    k.ACT(zz[i][:], zz[i][:], AF.Silu, [], [b_oz[i]])
                    k.TT(rs[:], rs[:], os_[i][:], ALU.mult, [b_oz[i]], [b_rs])
                    k.STT(ybT[:, hh, :], rs[:], dnwt[:, 0:1], zz[i][:], ALU.mult, ALU.mult, [b_rs, b_oz[i], b_c], [b_yb])
                for m in range(16):
                    i = m % 2
                    ms = slice(m * 128, (m + 1) * 128)
                    k.DMA(wbf[i][:, :, 0:128], wa[m], [], [b_wbf[i]], q='pool')
                    k.DMA(wbf[i][:, :, 128:256], wb[m], [], [b_wbf[i]], q='pool')
                    k.DMA(gt[i][:, 0, :], gav[:, m, 64 + t0:64 + t0 + 512], [bHW], [b_gt[i]], q='sp')
                    k.DMA(gt[i][:, 1, :], gbv[:, m, 64 + t0:64 + t0 + 512], [bHW], [b_gt[i]], q='pool')
                    k.ACT(gt[i][:], gt[i][:], AF.Sigmoid, [], [b_gt[i]])
                    for c in range(8):
                        k.MM(pA[:], wbf[i][:, c, 0:128], yaT[:, c, t0:t0 + 512], [b_wbf[i], b_ya], [b_pA], start=(c == 0), stop=(c == 7))
                    for c in range(8):
                        k.MM(pB[:], wbf[i][:, c, 128:256], ybT[:, c, :], [b_wbf[i], b_yb], [b_pB], start=(c == 0), stop=(c == 7))
                    k.TT(t1[:], pA[:], gt[i][:, 0, :], ALU.mult, [b_pA, b_gt[i]], [b_t])
                    k.TT(t2[:], pB[:], gt[i][:, 1, :], ALU.mult, [b_pB, b_gt[i]], [b_t])
                    k.TT(mg[:, m, :], t1[:], t2[:], ALU.add, [b_t], [b_mg], eng='pool')
                for dc in range(16):
                    i = dc % 2
                    k.DMA(wbo[i][:], wo[dc], [], [b_wbo[i]], q='pool')
                    k.DMA(xs[i][:], XTv[:, dc, 64 + t0:64 + t0 + 512], [T['b_XT']], [b_xs[i]])
                    for m in range(16):
                        k.MM(pY[i][:], wbo[i][:, m, :], mg[:, m, :], [b_mg, b_wbo[i]], [b_pY[i]], start=(m == 0), stop=(m == 15))
                    k.STT(ys[i][:], pY[i][:], g1t[:, dc:dc + 1], xs[i][:], ALU.mult, ALU.add, [b_pY[i], b_c, b_xs[i]], [b_ys[i]])
                    k.DMA(X1v[:, dc, t0:t0 + 512], ys[i][:], [b_ys[i]], [T['b_X1T']], q='pool')
            k.wait_all('sp', [T['b_X1T']])
            k.emit()


def phase_D(nc, k, T, st, nblk=NBLK):
    I = T['I']
    n2w = I("n2w", [128, 16]); fnw = I("fnw", [128, 16]); wq = I("wq", [16, 128, 16, 128]); skT = I("skT", [128, 2, 128])
    uT = I("uT", [128, 128, 16, 128]); v = I("v", [16384, 2048]); iota = I("iota", [128, 128]); ident = T['ident']
    x1T, MOD, out = T['X1T'], T['MOD'], T['out']
    UB, VB = T['UB'], T['VB']
    b_UB = [Buf() for _ in range(128)]; b_VB = [[Buf() for _ in range(128)] for _ in range(2)]
    if True:
        sb = lambda n, s, dt=F32: st.enter_context(nc.sbuf_tensor("d_" + n, s, dt))
        b_out = Buf(); b_c = Buf()
        P = [st.enter_context(nc.psum_tensor(f"d_P{i}", [128, 512], F32)) for i in range(8)]
        bP = [Buf() for _ in range(8)]
        ones = sb("ones", [128, 128]); n2t = sb("n2t", [128, 16]); m2t = sb("m2t", [128, 3, 16]); fnt = sb("fnt", [128, 16])
        skt = sb("skt", [128, 2, 128]); iot = sb("iot", [128, 128]); iob = sb("iob", [128, 128], BF16); idt = sb("idt", [128, 128])
        scl2 = sb("scl2", [128, 16])
        k.MSET(ones[:], 1.0, [], [b_c])
        for dst, src in [(n2t, n2w), (fnt, fnw), (skt, skT), (iot, iota), (idt, ident)]:
            k.DMA(dst[:], src, [], [b_c])
        for q_ in range(3):
            k.DMA(m2t[:, q_, :], MOD[:, 0, 48 + 16 * q_:64 + 16 * q_], [T['b_MOD']], [b_c])
        k.CP(iob[:], iot[:], [b_c], [b_c])
        k.STT(scl2[:], m2t[:, 1, :], 1.0, n2t[:], ALU.add, ALU.mult, [b_c], [b_c])
        xb = sb("xb", [128, 16, TB]); b_xb = Buf()
        sq = sb("sq", [128, TB]); b_sq = Buf(); rstd = sb("rstd", [128, TB]); b_rstd = Buf()
        h2 = sb("h2", [128, 16, TB], BF16); b_h2 = Buf()
        wbf = [sb(f"wbf{i}", [128, 16, 128], BF16) for i in range(4)]; b_wbf = [Buf() for _ in range(4)]
        qT = sb("qT", [128, 16, TB]); b_qT = Buf()
        sc = [sb(f"sc{i}", [128, 16, 128]) for i in range(2)]; scw = [sb(f"scw{i}", [128, 128]) for i in range(2)]; b_sc = [Buf(), Buf()]; b_scw = [Buf(), Buf()]
        tv = [sb(f"tv{i}", [128, 16, 16]) for i in range(2)]; ti = [sb(f"ti{i}", [128, 16, 16], U32) for i in range(2)]
        tif = [sb(f"tif{i}", [128, 16, 16]) for i in range(2)]; b_tv = [Buf(), Buf()]
        cand = [sb(f"cand{i}", [128, 8, 256]) for i in range(2)]; cw_ = [sb(f"candw{i}", [128, 256]) for i in range(2)]; b_cand = [Buf(), Buf()]; b_cw = [Buf(), Buf()]
        bs = [sb(f"bs{i}", [128, 8, 16]) for i in range(2)]; bp = [sb(f"bp{i}", [128, 8, 16], U32) for i in range(2)]; ba = sb("ba", [128, 8, 16], U32); bb = sb("bb", [128, 8, 16], U32)
        af = sb("af", [128, 8, 16]); bf = sb("bf", [128, 8, 16]); b_bs = [Buf(), Buf()]; b_bab = Buf()
        eq = sb("eq", [128, 8, 16, 16]); b_eq = Buf()
        ijw = sb("ijw", [128, 3, 8, 16]); b_ijw = Buf()
        ssum = sb("ssum", [128, 8]); b_ss = Buf()
        IJW = sb("IJW", [128, 3, TB]); b_IJW = Buf()
        NB = 16
        oh = [sb(f"oh{i}", [128, 2, NB, 128], BF16) for i in range(2)]; b_oh = [Buf(), Buf()]
        CT = sb("CT", [128, 128, TB], BF16); b_CT = Buf()
        gl = [sb(f"gl{i}", [128, TB]) for i in range(2)]; b_gl = [Buf(), Buf()]
        scb = [sc[i_][:].rearrange("p a b -> p (a b)").bitcast(BF16) for i_ in range(2)]
        vbf = [scb[0][:, 0:2048], scb[0][:, 2048:4096], scb[1][:, 0:2048], scb[1][:, 2048:4096]]; b_vbf = [Buf() for _ in range(4)]
        dmy = sb("dmy", [128, 8])
        b_orow = [Buf(), Buf()]
        xv = x1T.rearrange("(c p) t -> p c t", p=128)
        for blk in range(nblk):
            t0 = blk * TB
            k.DMA(xb[:], xv[:, :, t0:t0 + TB], [T['b_X1T']], [b_xb])
            for c in range(16):
                k.ACT(sq[:], xb[:, c, :], AF.Square, [b_xb], [b_sq])
                k.MM(P[0][:, 0:TB], ones[:], sq[:], [b_sq, b_c], [bP[0]], start=(c == 0), stop=(c == 15))
            k.TS(rstd[:], P[0][:, 0:TB], 1.0 / 2048, 1e-6, ALU.mult, ALU.add, [bP[0]], [b_rstd])
            k.ACT(rstd[:], rstd[:], AF.Sqrt, [], [b_rstd])
            k.RCP(rstd[:], rstd[:], [], [b_rstd])
            for c in range(16):
                k.TT(sq[:], xb[:, c, :], rstd[:], ALU.mult, [b_xb, b_rstd], [b_sq])
                k.ACT(h2[:, c, :], sq[:], AF.Identity, [b_sq, b_c], [b_h2], scale=scl2[:, c:c + 1], bias=m2t[:, 0, c:c + 1])
            for cq in range(16):
                i = cq % 2
                k.DMA(wbf[i][:], wq[cq], [], [b_wbf[i]], q='pool')
                pb = 1 + (cq % 2)
                for c in range(16):
                    k.MM(P[pb][:, 0:TB], wbf[i][:, c, :], h2[:, c, :], [b_wbf[i], b_h2], [bP[pb]], start=(c == 0), stop=(c == 15))
                k.CP(qT[:, cq, :], P[pb][:, 0:TB], [bP[pb]], [b_qT], eng='act')
            NTT = TB // 128
            for tt in range(NTT):
                ts_ = slice(tt * 128, (tt + 1) * 128)
                for cq in range(16):
                    pb = 3 + (cq // 4) % 2
                    k.MM(P[pb][:, (cq % 4) * 128:(cq % 4 + 1) * 128], qT[:, cq, ts_], skt[:, cq % 2, :], [b_qT, b_c], [bP[pb]])
                    if cq % 4 == 3:
                        k.CP(sc[tt][:, cq - 3:cq + 1, :], P[pb][:].rearrange("p (a b) -> p a b", b=128), [bP[pb]], [b_sc[tt]], eng='act')
            for cq in range(16):
                for tt in range(NTT):
                    k.dve(lambda e, cq=cq, tt=tt: e.max(out=tv[tt][:, cq, 0:8], in_=sc[tt][:, cq, :]), [b_sc[tt]], [b_tv[tt]])
                    k.dve(lambda e, cq=cq, tt=tt: e.max_index(out=ti[tt][:, cq, 0:8], in_max=tv[tt][:, cq, 0:8], in_values=sc[tt][:, cq, :]),
                          [b_sc[tt]], [b_tv[tt]])
                    k.dve(lambda e, cq=cq, tt=tt: e.match_replace(out=scw[tt][:], in_to_replace=tv[tt][:, cq, 0:8], in_values=sc[tt][:, cq, :],
                                                                  imm_value=NEG), [b_sc[tt], b_tv[tt]], [b_scw[tt]])
                    k.dve(lambda e, cq=cq, tt=tt: e.max(out=tv[tt][:, cq, 8:16], in_=scw[tt][:]), [b_scw[tt]], [b_tv[tt]])
                    k.dve(lambda e, cq=cq, tt=tt: e.max_index(out=ti[tt][:, cq, 8:16], in_max=tv[tt][:, cq, 8:16], in_values=scw[tt][:]),
                          [b_scw[tt]], [b_tv[tt]])
            for tt in range(NTT):
                k.CP(tif[tt][:], ti[tt][:], [b_tv[tt]], [b_tv[tt]])
                for h in range(8):
                    k.TT(cand[tt][:, h, :].rearrange("p (a b) -> p a b", b=16), tv[tt][:, 2 * h, :].unsqueeze(2).broadcast_to([128, 16, 16]),
                         tv[tt][:, 2 * h + 1, :].unsqueeze(1).broadcast_to([128, 16, 16]), ALU.add, [b_tv[tt]], [b_cand[tt]], eng='pool')
            for h in range(8):
                for tt in range(NTT):
                    k.dve(lambda e, h=h, tt=tt: e.max(out=bs[tt][:, h, 0:8], in_=cand[tt][:, h, :]), [b_cand[tt]], [b_bs[tt]])
                    k.dve(lambda e, h=h, tt=tt: e.max_index(out=bp[tt][:, h, 0:8], in_max=bs[tt][:, h, 0:8], in_values=cand[tt][:, h, :]),
                          [b_cand[tt]], [b_bs[tt]])
                    k.dve(lambda e, h=h, tt=tt: e.match_replace(out=cw_[tt][:], in_to_replace=bs[tt][:, h, 0:8], in_values=cand[tt][:, h, :],
                                                                imm_value=NEG), [b_cand[tt], b_bs[tt]], [b_cw[tt]])
                    k.dve(lambda e, h=h, tt=tt: e.max(out=bs[tt][:, h, 8:16], in_=cw_[tt][:]), [b_cw[tt]], [b_bs[tt]])
                    k.dve(lambda e, h=h, tt=tt: e.max_index(out=bp[tt][:, h, 8:16], in_max=bs[tt][:, h, 8:16], in_values=cw_[tt][:]),
                          [b_cw[tt]], [b_bs[tt]])
            for tt in range(NTT):
                ts_ = slice(tt * 128, (tt + 1) * 128)
                tif4 = tif[tt][:].rearrange("p (h two) a -> p h two a", two=2)
                k.TS(ba[:], bp[tt][:], 4, None, ALU.logical_shift_right, None, [b_bs[tt]], [b_bab])
                k.TS(bb[:], bp[tt][:], 15, None, ALU.bitwise_and, None, [b_bs[tt]], [b_bab])
                k.CP(af[:], ba[:], [], [b_bab]); k.CP(bf[:], bb[:], [], [b_bab])
                for which, sel in ((0, af), (1, bf)):
                    k.TT(eq[:], sel[:].unsqueeze(3).broadcast_to([128, 8, 16, 16]),
                         iot[:, 0:16].unsqueeze(1).unsqueeze(1).broadcast_to([128, 8, 16, 16]), ALU.is_equal, [b_bab, b_c], [b_eq])
                    k.TT(eq[:], eq[:], tif4[:, :, which, :].unsqueeze(2).broadcast_to([128, 8, 16, 16]), ALU.mult, [b_tv[tt]], [b_eq])
                    k.dve(lambda e, which=which: e.tensor_reduce(out=ijw[:, which, :, :], in_=eq[:], op=ALU.add, axis=mybir.AxisListType.X),
                          [b_eq], [b_ijw])
                k.TT(ijw[:, 2, :, :], bs[tt][:], bs[tt][:, :, 0:1].broadcast_to([128, 8, 16]), ALU.subtract, [b_bs[tt]], [b_ijw])
                k.ACT(ijw[:, 2, :, :], ijw[:, 2, :, :], AF.Exp, [], [b_ijw])
                k.dve(lambda e: e.tensor_reduce(out=ssum[:], in_=ijw[:, 2, :, :], op=ALU.add, axis=mybir.AxisListType.X), [b_ijw], [b_ss])
                k.RCP(ssum[:], ssum[:], [], [b_ss])
                k.TT(ijw[:, 2, :, :], ijw[:, 2, :, :], ssum[:].unsqueeze(2).broadcast_to([128, 8, 16]), ALU.mult, [b_ss], [b_ijw])
                for q_ in range(3):
                    k.TR(P[5][:, q_ * 128:(q_ + 1) * 128], ijw[:, q_, :, :].rearrange("p h k -> p (h k)"), idt[:], [b_ijw, b_c], [bP[5]])
                k.CP(IJW[:, :, ts_], P[5][:, 0:384].rearrange("p (a b) -> p a b", b=128), [bP[5]], [b_IJW], eng='act')
            for g in range(TB // NB):
                i = g % 2
                g0 = g * NB
                k.TT(oh[i][:, 0, :, :], iob[:].unsqueeze(1).broadcast_to([128, NB, 128]),
                     IJW[:, 1, g0:g0 + NB].unsqueeze(2).broadcast_to([128, NB, 128]), ALU.is_equal, [b_IJW, b_c], [b_oh[i]])
                k.TT(oh[i][:, 1, :, :], iob[:].unsqueeze(1).broadcast_to([128, NB, 128]),
                     IJW[:, 0, g0:g0 + NB].unsqueeze(2).broadcast_to([128, NB, 128]), ALU.is_equal, [b_IJW, b_c], [b_oh[i]])
                k.TT(oh[i][:, 1, :, :], oh[i][:, 1, :, :], IJW[:, 2, g0:g0 + NB].unsqueeze(2).broadcast_to([128, NB, 128]), ALU.mult,
                     [b_IJW], [b_oh[i]])
                for q4 in range(NB // 4):
                    pb = 6 + (q4 % 2)
                    for s_ in range(4):
                        tk = q4 * 4 + s_
                        k.MM(P[pb][:, s_ * 128:(s_ + 1) * 128], oh[i][:, 0, tk, :], oh[i][:, 1, tk, :], [b_oh[i]], [bP[pb]])
                    tg = g0 + q4 * 4
                    k.CP(CT[:, :, tg:tg + 4].rearrange("p i t -> p t i"), P[pb][:].rearrange("p (t i) -> p t i", i=128), [bP[pb]], [b_CT],
                         eng='act' if q4 % 2 == 0 else 'dve')
            for ie in range(128):
                i = ie % 4
                j2 = ie % 2
                if blk == 0:
                    k.DMA(wbf[i][:], uT[ie], [], [b_wbf[i]], q='pool')
                    if nblk > 1:
                        k.DMA(UB[ie], wbf[i][:], [b_wbf[i]], [b_UB[ie]], q='sp')
                else:
                    k.DMA(wbf[i][:], UB[ie], [b_UB[ie]], [b_wbf[i]], q='sp' if ie % 2 == 0 else 'pool')
                for c in range(16):
                    k.MM(P[j2][:, 0:TB], wbf[i][:, c, :], h2[:, c, :], [b_wbf[i], b_h2], [bP[j2]], start=(c == 0), stop=(c == 15))
                k.ACT(gl[j2][:], P[j2][:, 0:TB], AF.Gelu, [bP[j2]], [b_gl[j2]])
                k.TT(CT[:, ie, :], gl[j2][:], CT[:, ie, :], ALU.mult, [b_gl[j2]], [b_CT], eng='dve' if j2 == 0 else 'pool')
            k.pool(lambda e: e.memset(dmy[:], 0.0), [b_sc[0], b_sc[1], b_scw[0], b_scw[1], b_tv[0], b_tv[1]], b_vbf)
            for ie in range(128):
                i = ie % 4
                vsl = (slice(ie * 128, (ie + 1) * 128), slice(0, 2048))
                if blk == 0:
                    k.DMA(vbf[i], v[vsl], [], [b_vbf[i]], q='pool')
                    if nblk > 1:
                        k.DMA(VB[vsl], vbf[i], [b_vbf[i]], [b_VB[0][ie]], q='sp')
                else:
                    k.DMA(vbf[i], VB[vsl], [b_VB[0][ie]], [b_vbf[i]], q='sp' if ie % 2 == 0 else 'pool')
                for dc in range(16):
                    k.mm(lambda e, i=i, dc=dc, ie=ie: e.matmul(P[dc // 2][:, (dc % 2) * TB:(dc % 2 + 1) * TB], lhsT=vbf[i][:, dc * 128:(dc + 1) * 128],
                                                          rhs=CT[:, ie, :], start=(ie == 0 and dc % 2 == 0), stop=(ie == 127),
                                                          skip_group_check=True), [b_vbf[i], b_CT], [bP[dc // 2]])
            for c in range(16):
                k.STT(xb[:, c, :], P[c // 2][:, (c % 2) * TB:(c % 2 + 1) * TB], m2t[:, 2, c:c + 1], xb[:, c, :], ALU.mult, ALU.add,
                      [bP[c // 2], b_c], [b_xb])
            k.pool(lambda e: e.memset(dmy[:], 0.0), b_vbf, [b_sc[0], b_sc[1]])
            for c in range(16):
                k.ACT(sq[:], xb[:, c, :], AF.Square, [b_xb], [b_sq])
                k.MM(P[0][:, 0:TB], ones[:], sq[:], [b_sq, b_c], [bP[0]], start=(c == 0), stop=(c == 15))
            k.TS(rstd[:], P[0][:, 0:TB], 1.0 / 2048, 1e-6, ALU.mult, ALU.add, [bP[0]], [b_rstd])
            k.ACT(rstd[:], rstd[:], AF.Sqrt, [], [b_rstd])
            k.RCP(rstd[:], rstd[:], [], [b_rstd])
            for c in range(16):
                k.STT(xb[:, c, :], xb[:, c, :], fnt[:, c:c + 1], rstd[:], ALU.mult, ALU.mult, [b_rstd, b_c], [b_xb])
            for tt in range(TB // 128):
                i = tt % 2
                for c in range(16):
                    pb = (c // 4) % 2
                    k.TR(P[pb][:, (c % 4) * 128:(c % 4 + 1) * 128], xb[:, c, tt * 128:(tt + 1) * 128], idt[:], [b_xb, b_c], [bP[pb]])
                    if c % 4 == 3:
                        k.CP(qT[:, 8 * i:8 * i + 8, :].rearrange("p a b -> p (a b)")[:, (c - 3) * 128:(c + 1) * 128], P[pb][:], [bP[pb]], [b_orow[i], b_qT],
                             eng='act' if pb == 0 else 'dve')
                k.DMA(out[t0 + tt * 128:t0 + (tt + 1) * 128, :], qT[:, 8 * i:8 * i + 8, :].rearrange("p a b -> p (a b)"), [b_orow[i], b_qT], [b_out],
                      q='sp' if i == 0 else 'pool')
        k.wait_all('sp', [b_out])
        k.emit()


def build_F(upto=4):
    nc = bass.Bass('TRN2', target_bir_lowering=False)
    ins = {}

    def I(n, s, dt=F32):
        if n not in ins:
            ins[n] = nc.dram_tensor(n, s, dt, kind="ExternalInput").ap()
        return ins[n]
    T = {'I': I, 'ins': ins}
    T['ident'] = I("ident", [128, 128]); T['oh4'] = I("oh4", [128, 4])
    T['out'] = nc.dram_tensor("out", [NQ, D], F32, kind="ExternalOutput").ap()

    def scr(name, shape, dt=F32):
        T[name] = nc.dram_tensor("scr_" + name, shape, dt).ap()
        T['b_' + name] = Buf()
    scr('XT', [D, NTOK]); scr('HW', [MCH * 128, NTOK]); scr('MOD', [128, 2, 96])
    scr('G1i', [4 * GR, NTOK], BF16); scr('G1o', [4 * GR, NTOK], BF16); scr('G1fi', [128, NTOK]); scr('G1fo', [128, NTOK])
    scr('GKi', [1024, NT], BF16); scr('GKo', [1024, NT], BF16)
    scr('GVi', [4 * 2 * NTL * 128, 128], BF16); scr('GVo', [4 * 2 * NTL * 128, 128], BF16)
    scr('GOi', [1024, 8192], BF16); scr('GOo', [1024, 8192], BF16)
    scr('KR', [64, NT], BF16); scr('X1T', [D, NQ])
    scr('UB', [128, 128, 16, 128], BF16); scr('VB', [16384, 2048], BF16)
    with ExitStack() as st0:
        k = Ctx(nc, st0)
        phase_A(nc, k, T)
        T['b_G1o'] = [Buf() for _ in range(27)]
        first = sorted({(s_ * GR + r_) // 512 for s_ in range(4) for r_ in (0, 319)})
        for c_ in first + [c_ for c_ in range(27) if c_ not in first]:
            k.coll(lambda e, c_=c_: e.collective_compute("AllReduce", ALU.add, replica_groups=RG, ins=[T['G1i'][c_ * 512:(c_ + 1) * 512, :].opt()],
                                                         outs=[T['G1o'][c_ * 512:(c_ + 1) * 512, :].opt()]), [T['b_G1i']], [T['b_G1o'][c_]])
        allreduce_chunks(k, T['G1fi'], T['G1fo'], 128, 128, [T['b_G1fi']], [T['b_G1fo']])
        if upto >= 2:
            phase_B(nc, k, T)
            for nm in ('GKo', 'GVo', 'GOo'):
                T['b_' + nm] = [Buf() for _ in range(8)]
            for h in range(8):
                for nm, ch in (('GK', 128), ('GV', NT)):
                    k.coll(lambda e, nm=nm, ch=ch, h=h: e.collective_compute(
                        "AllReduce", ALU.add, replica_groups=RG, ins=[T[nm + 'i'][h * ch:(h + 1) * ch, :].opt()],
                        outs=[T[nm + 'o'][h * ch:(h + 1) * ch, :].opt()]), [T['b_' + nm + 'i']], [T['b_' + nm + 'o'][h]])
            for h in range(8):
                k.coll(lambda e, h=h: e.collective_compute(
                    "AllReduce", ALU.add, replica_groups=RG, ins=[T['GOi'][h * 128:(h + 1) * 128, :].opt()],
                    outs=[T['GOo'][h * 128:(h + 1) * 128, :].opt()]), [T['b_GOi']], [T['b_GOo'][h]])
        if upto >= 3:
            with ExitStack() as stc:
                phase_C(nc, k, T, stc)
        if upto >= 4:
            with ExitStack() as std:
                phase_D(nc, k, T, std)
        else:
            k.wait_all('pool', T['b_G1o'] + [T['b_G1fo']] + T['b_GKo'] + T['b_GVo'] + T['b_GOo'])
            k.emit()
    return nc, T


def host_F(inp):
    cos, sin = rope_tables()
    COSf = np.concatenate([np.ones((64, 256), np.float32), cos], 1)
    SINf = np.concatenate([np.zeros((64, 256), np.float32), sin], 1)
    masks, ident, bmask = gdn_consts()
    fm = lambda vec: np.ascontiguousarray(vec.reshape(16, 128).T)

    def chunked(w):
        K, N = w.shape
        Np = -(-N // 128) * 128
        if Np != N:
            w = np.concatenate([w, np.zeros((K, Np - N), w.dtype)], 1)
        return np.ascontiguousarray(w.reshape(K // 128, 128, Np // 128, 128).transpose(2, 1, 0, 3))
    wuq = inp['w_uq'][0]
    wuqn = np.ascontiguousarray(np.stack([wuq[:, h * 192:h * 192 + 128] for h in range(8)]))
    wuqr = np.ascontiguousarray(np.stack([wuq[:, h * 192 + 128:h * 192 + 192] for h in range(8)]))
    wuqp = np.ascontiguousarray(wuqr[:, :, PERM])
    sk = inp['peer_sub_keys'][0]
    common = {
        "w_mod": chunked(inp['w_mod'][0]), "b_mod": np.ascontiguousarray(inp['b_mod'][0].reshape(96, 128).T),
        "n1w": fm(inp['norm1_w'][0]), "w_in": chunked(inp['w_in'][0]), "ident": ident,
        "COS": COSf, "SIN": SINf, "kvw": np.ascontiguousarray(inp['mla_kv_norm_w'][0].reshape(2, 128).T),
        "masks": masks, "bmask": bmask, "qnw": np.ascontiguousarray(inp['mla_q_norm_w'][0].reshape(4, 128).T),
        "wuqn": wuqn, "wuqr": wuqr, "wuqp": wuqp, "dnw": np.ascontiguousarray(inp['dn_norm_w'][0].reshape(128, 1)),
        "wa": chunked(inp['w_branch_a'][0]), "wb": chunked(inp['w_branch_b'][0]),
        "wo": chunked(inp['w_out'][0]), "n2w": fm(inp['norm2_w'][0]), "fnw": fm(inp['final_norm_w']),
        "wq": chunked(inp['peer_w_q'][0]), "skT": np.ascontiguousarray(sk.transpose(2, 0, 1)),
        "uT": chunked(inp['peer_u'][0].T), "v": np.ascontiguousarray(inp['peer_v'][0]),
        "iota": np.ascontiguousarray(np.broadcast_to(np.arange(128, dtype=np.float32), (128, 128))),
    }
    maps = []
    for core in range(8):
        b, j = core // 4, core % 4
        tok = slice(2048 * j, 2048 * j + 2048)
        cw = inp['dn_conv_w'][0]
        convw = np.stack([cw[:, g * 1024 + (2 * j + hl) * 128: g * 1024 + (2 * j + hl + 1) * 128].T for hl in range(2) for g in range(3)], 1)
        al = np.array([inp['dn_a_log'][0, d, 2 * j + hl] for d in range(2) for hl in range(2)], np.float32)
        dtb = np.array([inp['dn_dt_bias'][0, d, 2 * j + hl] for d in range(2) for hl in range(2)], np.float32)
        m = dict(common)
        m.update({
            "x_own": np.ascontiguousarray(np.concatenate([inp['ctx'][b, 64 * j:64 * j + 64], inp['x'][b, tok]], 0)),
            "cc": np.ascontiguousarray(np.stack([inp['c'][b], inp['c_ctx']], -1).reshape(16, 128, 2).transpose(1, 0, 2)),
            "oh4": np.ascontiguousarray(np.broadcast_to(np.eye(4, dtype=np.float32)[j], (128, 4))),
            "wukv": np.ascontiguousarray(inp['w_ukv'][0][:, 2 * j * 256:(2 * j + 2) * 256]),
            "convw": np.ascontiguousarray(convw), "alog": np.ascontiguousarray(np.broadcast_to(al, (128, 4))),
            "dtb": np.ascontiguousarray(np.broadcast_to(dtb, (128, 4))),
            "COSq": np.ascontiguousarray(cos[:, tok]), "SINq": np.ascontiguousarray(sin[:, tok]),
        })
        maps.append(m)
    return maps


_CORES = list(range(8))


def kernel(**inputs):
    inp = {k_: np.asarray(v_) for k_, v_ in inputs.items()}
    nc, T = build_F()
    maps = [{k_: v_ for k_, v_ in m.items() if k_ in T['ins']} for m in host_F(inp)]
    res = run_bass_kernel_spmd(nc, maps, core_ids=_CORES).results
    out = np.empty((2, 8192, 2048), np.float32)
    for core in range(8):
        b, j = core // 4, core % 4
        out[b, 2048 * j:2048 * j + 2048, :] = np.asarray(res[core]["out"])
    return out
```
